# Optimizing a Trainium2 kernel written in Bass

```python
import jax, jax.numpy as jnp
from jax import lax
import numpy as np

D_MODEL = 1024
BATCH = 16
SEQ = 2048
DEPTH = 4

N_MIXERS = 2
N_CONV_LAYERS = (DEPTH + 1) // 2
N_MLA_LAYERS = DEPTH // 2
N_DENSE_LAYERS = (DEPTH + 1) // 2
N_MOE_LAYERS = DEPTH // 2
CONV_WIDTH = 3
N_HEADS = 8
QK_NOPE_DIM = 128
QK_ROPE_DIM = 64
QK_HEAD_DIM = QK_NOPE_DIM + QK_ROPE_DIM
V_HEAD_DIM = 128
Q_LORA_RANK = 384
KV_LORA_RANK = 256
ROPE_THETA = 10000.0
Q_BLOCK = 128
D_FF_DENSE = 2816
N_EXPERTS = 8
TOP_K = 2
D_FF_EXPERT = 2048
EPS = 1e-6

kernel_name = "hybrid_shortconv_mla_moe_trunk"


def rmsnorm(x, g):
    xf = x.astype(jnp.float32)
    y = xf * lax.rsqrt(jnp.mean(xf * xf, axis=-1, keepdims=True) + EPS)
    return (y * g.astype(jnp.float32)).astype(x.dtype)


def rope_tables(positions):
    inv_freq = ROPE_THETA ** (-jnp.arange(0, QK_ROPE_DIM, 2, dtype=jnp.float32) / QK_ROPE_DIM)
    ang = positions.astype(jnp.float32)[..., None] * inv_freq
    return jnp.cos(ang)[:, :, None, :], jnp.sin(ang)[:, :, None, :]


def apply_rope(x, cos, sin):
    x1, x2 = jnp.split(x, 2, axis=-1)
    cos = cos.astype(x.dtype)
    sin = sin.astype(x.dtype)
    return jnp.concatenate([x1 * cos - x2 * sin, x2 * cos + x1 * sin], axis=-1)


def short_conv_mixer(h, w_in, w_conv, w_out):
    seq = h.shape[1]
    b_gate, c_gate, u = jnp.split(h @ w_in, 3, axis=-1)
    v = c_gate * u
    vp = jnp.pad(v, ((0, 0), (CONV_WIDTH - 1, 0), (0, 0)))
    conv = vp[:, 0:seq] * w_conv[0]
    for k in range(1, CONV_WIDTH):
        conv = conv + vp[:, k:k + seq] * w_conv[k]
    return (b_gate * conv) @ w_out


def mla_mixer(h, cos, sin, w_down, q_a_norm, kv_a_norm, w_uq, w_ukv, q_norm, k_norm, w_o):
    bsz, seq, _ = h.shape
    down = h @ w_down
    c_q = rmsnorm(down[..., :Q_LORA_RANK], q_a_norm)
    c_kv = rmsnorm(down[..., Q_LORA_RANK:Q_LORA_RANK + KV_LORA_RANK], kv_a_norm)
    k_rope = down[..., Q_LORA_RANK + KV_LORA_RANK:]
    q = (c_q @ w_uq).reshape(bsz, seq, N_HEADS, QK_HEAD_DIM)
    kv = (c_kv @ w_ukv).reshape(bsz, seq, N_HEADS, QK_NOPE_DIM + V_HEAD_DIM)
    k_nope, v = kv[..., :QK_NOPE_DIM], kv[..., QK_NOPE_DIM:]
    k_rope_h = jnp.broadcast_to(k_rope[:, :, None, :], (bsz, seq, N_HEADS, QK_ROPE_DIM))
    k = jnp.concatenate([k_nope, k_rope_h], axis=-1)
    q = rmsnorm(q, q_norm)
    k = rmsnorm(k, k_norm)
    q = jnp.concatenate([q[..., :QK_NOPE_DIM], apply_rope(q[..., QK_NOPE_DIM:], cos, sin)], axis=-1)
    k = jnp.concatenate([k[..., :QK_NOPE_DIM], apply_rope(k[..., QK_NOPE_DIM:], cos, sin)], axis=-1)
    scale = QK_HEAD_DIM ** -0.5
    neg = jnp.finfo(jnp.float32).min
    outs = []
    for blk in range(seq // Q_BLOCK):
        s0 = blk * Q_BLOCK
        end = s0 + Q_BLOCK
        scores = jnp.einsum('bqhd,bkhd->bhqk', q[:, s0:end], k[:, :end]).astype(jnp.float32) * scale
        mask = jnp.arange(end)[None, :] <= (s0 + jnp.arange(Q_BLOCK))[:, None]
        p = jax.nn.softmax(jnp.where(mask, scores, neg), axis=-1).astype(v.dtype)
        outs.append(jnp.einsum('bhqk,bkhd->bqhd', p, v[:, :end]))
    o = jnp.concatenate(outs, axis=1).reshape(bsz, seq, N_HEADS * V_HEAD_DIM)
    return o @ w_o


def swiglu(h, w_gate, w_up, w_down):
    return (jax.nn.silu(h @ w_gate) * (h @ w_up)) @ w_down


def moe_swiglu(h, router, w_gate, w_up, w_down):
    bsz, seq, d = h.shape
    ht = h.reshape(bsz * seq, d)
    probs = jax.nn.softmax((ht @ router).astype(jnp.float32), axis=-1)
    vals, idx = lax.top_k(probs, TOP_K)
    wts = vals / jnp.sum(vals, axis=-1, keepdims=True)
    gates = jnp.einsum('tk,tke->te', wts, jax.nn.one_hot(idx, N_EXPERTS, dtype=jnp.float32)).astype(h.dtype)
    out = jnp.zeros_like(ht)
    for e in range(N_EXPERTS):
        out = out + gates[:, e:e + 1] * swiglu(ht, w_gate[e], w_up[e], w_down[e])
    return out.reshape(bsz, seq, d)


def setup_inputs(seed: int = 0) -> dict:
    key = jax.random.key(seed)
    ks = jax.random.split(key, 24)
    f32 = jnp.float32
    res_scale = (2 * DEPTH) ** -0.5

    def w(k, shape, fan_in, extra=1.0):
        return jax.random.normal(k, shape, f32) * (fan_in ** -0.5) * extra

    def gain(k, shape):
        return 1.0 + 0.02 * jax.random.normal(k, shape, f32)

    x = jax.random.normal(ks[0], (BATCH, SEQ, D_MODEL), f32)
    start = jax.random.randint(ks[1], (BATCH, 1), 0, 4096, dtype=jnp.int32)
    positions = (start + jnp.arange(SEQ, dtype=jnp.int32)[None, :]).astype(jnp.int32)
    return {
        "x": x,
        "positions": positions,
        "norm_mix": gain(ks[2], (DEPTH, D_MODEL)),
        "norm_ffn": gain(ks[3], (DEPTH, D_MODEL)),
        "conv_w_in": w(ks[4], (N_CONV_LAYERS, D_MODEL, 3 * D_MODEL), D_MODEL),
        "conv_w": w(ks[5], (N_CONV_LAYERS, CONV_WIDTH, D_MODEL), CONV_WIDTH),
        "conv_w_out": w(ks[6], (N_CONV_LAYERS, D_MODEL, D_MODEL), D_MODEL, res_scale),
        "mla_w_down": w(ks[7], (N_MLA_LAYERS, D_MODEL, Q_LORA_RANK + KV_LORA_RANK + QK_ROPE_DIM), D_MODEL),
        "mla_q_a_norm": gain(ks[8], (N_MLA_LAYERS, Q_LORA_RANK)),
        "mla_kv_a_norm": gain(ks[9], (N_MLA_LAYERS, KV_LORA_RANK)),
        "mla_w_uq": w(ks[10], (N_MLA_LAYERS, Q_LORA_RANK, N_HEADS * QK_HEAD_DIM), Q_LORA_RANK),
        "mla_w_ukv": w(ks[11], (N_MLA_LAYERS, KV_LORA_RANK, N_HEADS * (QK_NOPE_DIM + V_HEAD_DIM)), KV_LORA_RANK),
        "mla_q_norm": gain(ks[12], (N_MLA_LAYERS, QK_HEAD_DIM)),
        "mla_k_norm": gain(ks[13], (N_MLA_LAYERS, QK_HEAD_DIM)),
        "mla_w_o": w(ks[14], (N_MLA_LAYERS, N_HEADS * V_HEAD_DIM, D_MODEL), N_HEADS * V_HEAD_DIM, res_scale),
        "ffn_w_gate": w(ks[15], (N_DENSE_LAYERS, D_MODEL, D_FF_DENSE), D_MODEL),
        "ffn_w_up": w(ks[16], (N_DENSE_LAYERS, D_MODEL, D_FF_DENSE), D_MODEL),
        "ffn_w_down": w(ks[17], (N_DENSE_LAYERS, D_FF_DENSE, D_MODEL), D_FF_DENSE, res_scale),
        "moe_router": w(ks[18], (N_MOE_LAYERS, D_MODEL, N_EXPERTS), D_MODEL),
        "moe_w_gate": w(ks[19], (N_MOE_LAYERS, N_EXPERTS, D_MODEL, D_FF_EXPERT), D_MODEL),
        "moe_w_up": w(ks[20], (N_MOE_LAYERS, N_EXPERTS, D_MODEL, D_FF_EXPERT), D_MODEL),
        "moe_w_down": w(ks[21], (N_MOE_LAYERS, N_EXPERTS, D_FF_EXPERT, D_MODEL), D_FF_EXPERT, res_scale),
    }


def reference(x, positions, norm_mix, norm_ffn, conv_w_in, conv_w, conv_w_out,
              mla_w_down, mla_q_a_norm, mla_kv_a_norm, mla_w_uq, mla_w_ukv,
              mla_q_norm, mla_k_norm, mla_w_o, ffn_w_gate, ffn_w_up, ffn_w_down,
              moe_router, moe_w_gate, moe_w_up, moe_w_down):
    cos, sin = rope_tables(positions)
    for i in range(DEPTH):
        j = i // 2
        h = rmsnorm(x, norm_mix[i])
        if i % N_MIXERS == 0:
            x = x + short_conv_mixer(h, conv_w_in[j], conv_w[j], conv_w_out[j])
        else:
            x = x + mla_mixer(h, cos, sin, mla_w_down[j], mla_q_a_norm[j], mla_kv_a_norm[j],
                              mla_w_uq[j], mla_w_ukv[j], mla_q_norm[j], mla_k_norm[j], mla_w_o[j])
        h = rmsnorm(x, norm_ffn[i])
        if i % 2 == 0:
            x = x + swiglu(h, ffn_w_gate[j], ffn_w_up[j], ffn_w_down[j])
        else:
            x = x + moe_swiglu(h, moe_router[j], moe_w_gate[j], moe_w_up[j], moe_w_down[j])
    return x
```

```python
import contextlib
import numpy as np
import concourse.bass as bass
import concourse.mybir as mybir
from concourse.bass_utils import run_bass_kernel_spmd

F32 = mybir.dt.float32
BF16 = mybir.dt.bfloat16
I32 = mybir.dt.int32
AF = mybir.ActivationFunctionType
ALU = mybir.AluOpType

D = 1024
S = 2048
DEPTH = 4
NTT = S // 512
DC = D // 128
TCH = S // 128
NH = 8
DFF = 2816
NE = 8
DFE = 2048
QL = 384
KVL = 256
ROPE = 64
EPS = 1e-6
N_CORES = 8
SEQ_PER_CORE = 2
CAPR = 2048
CAPM = 640
SPARSE = True
OVT = CAPM
SKIP_MIX = False
SKIP_FFN = False

GC_NMIX = 0
GC_NFFN = GC_NMIX + DEPTH * DC
GC_CONV = GC_NFFN + DEPTH * DC
GC_QA = GC_CONV + 2 * 3 * DC
GC_KVA = GC_QA + 2 * 3
GC_QN = GC_KVA + 2 * 2
GC_KN = GC_QN + 4
GC_FREQ = GC_KN + 4
GC_SIGN = GC_FREQ + 1
NG = GC_SIGN + 1


class Sem:
    __slots__ = ("h", "val")

    def __init__(self, h):
        self.h = h
        self.val = 0


class Buf:
    __slots__ = ("w", "r")

    def __init__(self):
        self.w = None
        self.r = {}


class Eng:
    def __init__(self, name, e, sem):
        self.name = name
        self.e = e
        self.sem = sem
        self.waited = {}
        self.is_pe = name == "pe"
        self.ring = []
        self.ri = 0


class Tile:
    __slots__ = ("t", "buf")

    def __init__(self, t):
        self.t = t
        self.buf = Buf()


class RR:
    def __init__(self, items):
        self.items = items
        self.i = 0

    def get(self):
        it = self.items[self.i % len(self.items)]
        self.i += 1
        return it


class KB:
    def __init__(self, nc, es):
        self.nc = nc
        self.es = es

        def sem(n):
            return Sem(es.enter_context(nc.semaphore(n)))

        self.PE = Eng("pe", nc.tensor, sem("s_pe"))
        self.DVE = Eng("dve", nc.vector, sem("s_dve"))
        self.ACT = Eng("act", nc.scalar, sem("s_act"))
        self.POOL = Eng("pool", nc.gpsimd, sem("s_pool"))
        self.SP = Eng("sp", nc.sync, sem("s_sp"))
        self.POOL.ring = [sem(f"s_dp{i}") for i in range(12)]
        self.SP.ring = [sem(f"s_ds{i}") for i in range(12)]
        self.engines = [self.PE, self.DVE, self.ACT, self.POOL, self.SP]
        self.n_ins = 0
        self.n_guard = 0
        self.swq = []

    def wait(self, E, tag):
        s, v = tag
        if v <= 0 or E.waited.get(s, 0) >= v:
            return
        E.e.wait_ge(s.h, v)
        E.waited[s] = v

    def deps(self, E, reads, writes):
        for b in reads:
            if b.w is not None:
                if not (b.w[0] is E.sem and E.is_pe):
                    self.wait(E, b.w)
        for b in writes:
            if b.w is not None:
                if not (b.w[0] is E.sem and E.is_pe):
                    self.wait(E, b.w)
            for s, v in b.r.items():
                if s is E.sem and E.name != "pool":
                    continue
                self.wait(E, (s, v))

    def commit(self, tag, reads, writes):
        s, v = tag
        for b in reads:
            if b.r.get(s, 0) < v:
                b.r[s] = v
        for b in writes:
            b.w = tag
            b.r = {}

    def op(self, E, fn, reads=(), writes=()):
        self.deps(E, reads, writes)
        ins = fn()
        E.sem.val += 1
        ins.then_inc(E.sem.h, 1)
        self.commit((E.sem, E.sem.val), reads, writes)
        self.n_ins += 1

    def dma(self, Q, out_ap, in_ap, reads=(), writes=()):
        s = Q.ring[Q.ri % len(Q.ring)]
        Q.ri += 1
        self.wait(Q, (s, s.val))
        if Q is self.POOL:
            shp = list(out_ap.shape)
            nd = 1
            for d_ in shp[:-1]:
                nd *= int(d_)
            nd = max(1, nd // 16) * 2
            while self.swq and sum(n for _, n in self.swq) + nd > 512:
                tag, _ = self.swq.pop(0)
                self.wait(Q, tag)
        self.deps(Q, reads, writes)
        ins = Q.e.dma_start(out=out_ap, in_=in_ap)
        s.val += 16
        ins.then_inc(s.h, 16)
        self.commit((s, s.val), reads, writes)
        if Q is self.POOL:
            self.swq.append(((s, s.val), nd))
        self.n_ins += 1

    def idma(self, out_ap, out_off, in_ap, in_off, reads=(), writes=()):
        Q = self.POOL
        s = Q.ring[Q.ri % len(Q.ring)]
        Q.ri += 1
        self.wait(Q, (s, s.val))
        self.deps(Q, reads, writes)
        ins = Q.e.indirect_dma_start(out=out_ap, out_offset=out_off, in_=in_ap, in_offset=in_off)
        s.val += 16
        ins.then_inc(s.h, 16)
        self.commit((s, s.val), reads, writes)
        self.n_ins += 1

    def mm(self, out_ap, out_buf, parts):
        E = self.PE
        reads = [b for p in parts for b in p[2]]
        self.deps(E, reads, [out_buf])
        n = len(parts)
        ins = None
        for i, (l, r, _) in enumerate(parts):
            ins = E.e.matmul(out_ap, lhsT=l, rhs=r, start=(i == 0), stop=(i == n - 1))
            self.n_ins += 1
        E.sem.val += 1
        ins.then_inc(E.sem.h, 1)
        self.commit((E.sem, E.sem.val), reads, [out_buf])

    def mm1(self, out_ap, out_buf, l, r, reads, start, stop):
        E = self.PE
        self.deps(E, reads, [out_buf])
        ins = E.e.matmul(out_ap, lhsT=l, rhs=r, start=start, stop=stop)
        E.sem.val += 1
        ins.then_inc(E.sem.h, 1)
        self.commit((E.sem, E.sem.val), reads, [out_buf])
        self.n_ins += 1

    def transpose(self, out_ap, out_buf, in_ap, ident_ap, reads):
        E = self.PE
        self.deps(E, reads, [out_buf])
        ins = E.e.transpose(out_ap, in_ap, ident_ap)
        E.sem.val += 1
        ins.then_inc(E.sem.h, 1)
        self.commit((E.sem, E.sem.val), reads, [out_buf])
        self.n_ins += 1

    def all_sems(self):
        out = []
        for E in self.engines:
            out.append((E, E.sem))
            for r in E.ring:
                out.append((E, r))
        return out

    def guarded(self, regs, thr, body):
        nc = self.nc
        self.barrier()
        before = {id(s): s.val for _, s in self.all_sems()}
        caches = {E.name: dict(E.waited) for E in self.engines}
        saved = (self.POOL.ring, self.POOL.ri, self.SP.ring, self.SP.ri)
        self.swq = []
        self.POOL.ring = [Sem(self.es.enter_context(nc.semaphore(f"s_g{self.n_guard}_{i}"))) for i in range(4)]
        self.POOL.ri = 0
        self.SP.ring = []
        self.n_guard += 1
        with nc.If(nc.snap(regs) > thr):
            body()
        self.POOL.ring, self.POOL.ri, self.SP.ring, self.SP.ri = saved
        self.swq = []
        after = {id(s): s.val for _, s in self.all_sems()}
        with nc.Else():
            for E in self.engines:
                d = after[id(E.sem)] - before[id(E.sem)]
                while d > 0:
                    c = min(d, 200)
                    E.e.sem_inc(E.sem.h, c)
                    d -= c
        for E in self.engines:
            E.waited = caches[E.name]
        self.barrier()

    def barrier(self):
        tags = []
        for E in self.engines:
            tags.append((E.sem, E.sem.val))
            for s in E.ring:
                tags.append((s, s.val))
        for E in self.engines:
            for t in tags:
                if t[0] is E.sem:
                    continue
                self.wait(E, t)

    def act(self, out, in_, func, reads, writes, **kw):
        self.op(self.ACT, lambda: self.nc.scalar.activation(out=out, in_=in_, func=func, **kw), reads, writes)

    def stt(self, out, in0, scalar, in1, op0, op1, reads, writes):
        self.op(self.DVE, lambda: self.nc.vector.scalar_tensor_tensor(
            out=out, in0=in0, scalar=scalar, in1=in1, op0=op0, op1=op1), reads, writes)

    def tt(self, E, out, in0, in1, op, reads, writes):
        self.op(E, lambda: E.e.tensor_tensor(out=out, in0=in0, in1=in1, op=op), reads, writes)

    def ts(self, E, out, in0, s1, s2, op0, op1, reads, writes):
        if s2 is None:
            self.op(E, lambda: E.e.tensor_scalar(out=out, in0=in0, scalar1=s1, scalar2=None, op0=op0), reads, writes)
        else:
            self.op(E, lambda: E.e.tensor_scalar(out=out, in0=in0, scalar1=s1, scalar2=s2, op0=op0, op1=op1),
                    reads, writes)

    def cp(self, E, out, in_, reads, writes):
        if E is self.ACT:
            self.op(E, lambda: self.nc.scalar.copy(out=out, in_=in_), reads, writes)
        else:
            self.op(E, lambda: E.e.tensor_copy(out=out, in_=in_), reads, writes)


def build_program(layer_ids, n_seq):
    nc = bass.Bass("TRN2", target_bir_lowering=False)

    def din(name, shape, dt=F32):
        return nc.dram_tensor(name, list(shape), dt, kind="ExternalInput").ap()

    x_d = din("x", [n_seq, S, D])
    pos_d = din("positions", [n_seq, S], I32)
    gcols_d = din("gcols", [128, NG])
    cst_d = din("cst", [128, 640])
    w_conv_in = din("conv_w_in", [2, D, 3 * D])
    w_conv_out = din("conv_w_out", [2, D, D])
    w_mdown = din("mla_w_down", [2, D, QL + KVL + ROPE])
    w_muq = din("mla_w_uq", [2, QL, NH * 192])
    w_mukv = din("mla_w_ukv", [2, KVL, NH * 256])
    w_mo = din("mla_w_o", [2, D, D])
    w_fg = din("ffn_w_gate", [2, D, DFF])
    w_fu = din("ffn_w_up", [2, D, DFF])
    w_fd = din("ffn_w_down", [2, DFF, D])
    w_rt = din("moe_router", [2, D, NE])
    w_eg = din("moe_w_gate", [2, NE, D, DFE])
    w_eu = din("moe_w_up", [2, NE, D, DFE])
    w_ed = din("moe_w_down", [2, NE, DFE, D])
    out_d = nc.dram_tensor("out", [n_seq, S, D], F32, kind="ExternalOutput").ap()

    HS_d = nc.dram_tensor("hs_scr", [NE * CAPR, D], BF16, kind="Internal").ap()
    YS_d = nc.dram_tensor("ys_scr", [NE * CAPM, D], F32, kind="Internal").ap()
    CNT_d = nc.dram_tensor("cnt_scr", [1, 1], I32, kind="Internal").ap()

    es = contextlib.ExitStack()
    with es:
        k = KB(nc, es)
        PE, DVE, ACT, POOL, SP = k.PE, k.DVE, k.ACT, k.POOL, k.SP

        uid = [0]

        def sb(name, shape, dt, stack=es):
            uid[0] += 1
            return stack.enter_context(nc.sbuf_tensor(f"{name}_{uid[0]}", list(shape), dt))

        XT = sb("XT", [128, DC, S], F32)
        XTb = [[Buf() for _ in range(NTT)] for _ in range(DC)]
        HT = sb("HT", [128, DC, S], BF16)
        HTb = [[Buf() for _ in range(NTT)] for _ in range(DC)]
        GC = Tile(sb("GC", [128, NG], F32))
        CST = Tile(sb("CST", [128, 640], F32))
        ONES = Tile(sb("ONES", [128, 128], F32))
        EPSC = Tile(sb("EPSC", [128, 1], F32))
        TRI = Tile(sb("TRI", [128, 128], BF16))
        psum = [Tile(es.enter_context(nc.psum_tensor(f"ps{i}", [128, 512], F32))) for i in range(8)]

        ident = CST.t[:, 0:128]

        def gcol(i, p0=0, p1=128):
            return GC.t[p0:p1, i:i + 1]

        k.dma(SP, GC.t[:], gcols_d, [], [GC.buf])
        k.dma(SP, CST.t[:], cst_d, [], [CST.buf])
        k.op(DVE, lambda: nc.vector.memset(ONES.t[:], 1.0), [], [ONES.buf])
        k.op(DVE, lambda: nc.vector.memset(EPSC.t[:], EPS), [], [EPSC.buf])
        k.cp(DVE, TRI.t[:], CST.t[:, 128:256], [CST.buf], [TRI.buf])
        sc = float(192.0 ** -0.5)
        k.ts(DVE, GC.t[:, GC_QN:GC_QN + 4], GC.t[:, GC_QN:GC_QN + 4], sc, None, ALU.mult, None, [GC.buf], [GC.buf])

        def rms_stats(stack_pools, srcs, nfeat):
            SQ, SSP, RT, RS = stack_pools
            ss = SSP.get()
            n = len(srcs)
            for i, (ap, b, p0, p1) in enumerate(srcs):
                sq = SQ.get()
                k.act(sq.t[p0:p1, :], ap, AF.Square, [b], [sq.buf])
                k.mm1(ss.t[:], ss.buf, ONES_B.t[p0:p1, :], sq.t[p0:p1, :], [ONES_B.buf, sq.buf], i == 0, i == n - 1)
            rt = RT.get()
            k.act(rt.t[:], ss.t[:], AF.Ln, [ss.buf, EPSC.buf], [rt.buf], scale=1.0 / nfeat, bias=EPSC.t[:])
            rs = RS.get()
            k.act(rs.t[:], rt.t[:], AF.Exp, [rt.buf], [rs.buf], scale=-0.5)
            return rs

        def main_norm(l, which, pools, hf_cb=None):
            gbase = (GC_NMIX if which == 0 else GC_NFFN) + l * DC

            def stats(tt):
                cols = slice(tt * 512, (tt + 1) * 512)
                return rms_stats(pools, [(XT[:, c, cols], XTb[c][tt], 0, 128) for c in range(DC)], D)

            rs_next = stats(0)
            for tt in range(NTT):
                cols = slice(tt * 512, (tt + 1) * 512)
                rs = rs_next
                if tt + 1 < NTT:
                    rs_next = stats(tt + 1)
                if hf_cb is None:
                    for c in range(DC):
                        k.stt(HT[:, c, cols], XT[:, c, cols], gcol(gbase + c), rs.t[:], ALU.mult, ALU.mult,
                              [XTb[c][tt], GC.buf, rs.buf], [HTb[c][tt]])
                else:
                    hf_cb(tt, cols, rs, gbase)

        def add_to_x(c, tt, ps):
            cols = slice(tt * 512, (tt + 1) * 512)
            k.tt(DVE, XT[:, c, cols], XT[:, c, cols], ps.t[:], ALU.add, [XTb[c][tt], ps.buf], [XTb[c][tt]])

        def tmp_pool(stack, name, n, dt=F32, shape=(128, 512)):
            return RR([Tile(sb(f"{name}{i}", list(shape), dt, stack)) for i in range(n)])

        def load_x(b):
            with contextlib.ExitStack() as st:
                XIN = tmp_pool(st, "xin", 4, F32, (128, D))
                PSP = RR(psum)
                for tc in range(TCH):
                    xin = XIN.get()
                    k.dma(SP, xin.t[:], x_d[b, tc * 128:(tc + 1) * 128, :], [], [xin.buf])
                    tt = tc // 4
                    for c in range(DC):
                        ps = PSP.get()
                        k.transpose(ps.t[:, 0:128], ps.buf, xin.t[:, c * 128:(c + 1) * 128], ident, [xin.buf, CST.buf])
                        E = DVE if c % 2 == 0 else ACT
                        k.cp(E, XT[:, c, tc * 128:(tc + 1) * 128], ps.t[:, 0:128], [ps.buf], [XTb[c][tt]])
                k.barrier()

        def store_x(b):
            with contextlib.ExitStack() as st:
                XO = tmp_pool(st, "xo", 4, F32, (128, D))
                PSP = RR(psum)
                for tc in range(TCH):
                    xo = XO.get()
                    tt = tc // 4
                    for c in range(DC):
                        ps = PSP.get()
                        k.transpose(ps.t[:, 0:128], ps.buf, XT[:, c, tc * 128:(tc + 1) * 128], ident,
                                    [XTb[c][tt], CST.buf])
                        E = DVE if c % 2 == 0 else ACT
                        k.cp(E, xo.t[:, c * 128:(c + 1) * 128], ps.t[:, 0:128], [ps.buf], [xo.buf])
                    k.dma(SP, out_d[b, tc * 128:(tc + 1) * 128, :], xo.t[:], [xo.buf], [])
                k.barrier()

        def conv_layer(l):
            j = l // 2
            with contextlib.ExitStack() as st:
                WIN = Tile(sb("c_win", [128, DC, 3 * D], BF16, st))
                WOUT = Tile(sb("c_wout", [128, DC, D], BF16, st))
                VB = Tile(sb("c_vb", [128, DC, 514], F32, st))
                GT = tmp_pool(st, "c_gt", 1, BF16, (128, DC, 512))
                SQ = tmp_pool(st, "c_sq", 2, BF16)
                RT = tmp_pool(st, "c_rt", 1)
                RS = tmp_pool(st, "c_rs", 2)
                CC = tmp_pool(st, "c_cc", 2)
                T0 = tmp_pool(st, "c_t0", 2)
                win_v = w_conv_in[j].rearrange("(c p) f -> p c f", p=128)
                WINb = [[Buf() for _ in range(2)] for _ in range(3)]
                for hf_ in range(2):
                    for g in range(3):
                        c0 = g * D + hf_ * 512
                        k.dma(POOL, WIN.t[:, :, c0:c0 + 512], win_v[:, :, c0:c0 + 512], [], [WINb[g][hf_]])
                wout_v = w_conv_out[j].rearrange("(c p) f -> p c f", p=128)
                for c in range(DC):
                    k.dma(POOL, WOUT.t[:, c, :], wout_v[:, c, :], [], [WOUT.buf])
                k.op(DVE, lambda: nc.vector.memset(VB.t[:, :, 0:2], 0.0), [], [VB.buf])
                main_norm(l, 0, (SQ, RR(psum[6:8]), RT, RS))
                PSB = RR(psum[0:6])
                PSY = RR(psum[6:8])
                cw = GC_CONV + j * 3 * DC
                for tt in range(NTT):
                    cols = slice(tt * 512, (tt + 1) * 512)
                    gt = GT.get()
                    for fc in range(DC):
                        pb, pc, pu = PSB.get(), PSB.get(), PSB.get()
                        for g, ps in enumerate((pb, pc, pu)):
                            k.mm(ps.t[:], ps.buf,
                                 [(WIN.t[:, kc, g * D + fc * 128: g * D + (fc + 1) * 128], HT[:, kc, cols],
                                   [WINb[g][fc // 4], HTb[kc][tt]]) for kc in range(DC)])
                        cc = CC.get()
                        k.cp(ACT, cc.t[:], pc.t[:], [pc.buf], [cc.buf])
                        k.tt(DVE, VB.t[:, fc, 2:514], cc.t[:], pu.t[:], ALU.mult, [cc.buf, pu.buf], [VB.buf])
                        t0 = T0.get()
                        k.act(t0.t[:], VB.t[:, fc, 2:514], AF.Copy, [VB.buf, GC.buf], [t0.buf],
                              scale=gcol(cw + 2 * DC + fc))
                        k.stt(t0.t[:], VB.t[:, fc, 1:513], gcol(cw + 1 * DC + fc), t0.t[:], ALU.mult, ALU.add,
                              [VB.buf, GC.buf, t0.buf], [t0.buf])
                        k.stt(t0.t[:], VB.t[:, fc, 0:512], gcol(cw + 0 * DC + fc), t0.t[:], ALU.mult, ALU.add,
                              [VB.buf, GC.buf, t0.buf], [t0.buf])
                        k.tt(DVE, gt.t[:, fc, :], t0.t[:], pb.t[:], ALU.mult, [t0.buf, pb.buf], [gt.buf])
                    k.cp(DVE, VB.t[:, :, 0:2], VB.t[:, :, 512:514], [VB.buf], [VB.buf])
                    for dm in range(DC):
                        py = PSY.get()
                        k.mm(py.t[:], py.buf,
                             [(WOUT.t[:, fc, dm * 128:(dm + 1) * 128], gt.t[:, fc, :], [WOUT.buf, gt.buf])
                              for fc in range(DC)])
                        add_to_x(dm, tt, py)
                k.barrier()

        def ffn_blocks(st, blocks, gate_mul=None, slots=None):
            if slots is None:
                GUS = [Tile(sb(f"f_gu{i}", [128, 8192], BF16, st)) for i in range(2)]
                WDS = [Tile(sb(f"f_wd{i}", [128, 4096], BF16, st)) for i in range(2)]
            else:
                GUS, WDS = slots
            AT = tmp_pool(st, "f_at", 2, BF16, (128, 4, 512))
            SS = tmp_pool(st, "f_ss", 2)
            TT = tmp_pool(st, "f_tt", 2)
            PSG = RR(psum[0:4])
            PSY = RR(psum[4:8])
            nb = len(blocks)
            units = [(i, tt) for i in range(nb) for tt in range(NTT)]
            gu_l, wd_l = {}, {}

            def load_gu(i):
                if i >= nb or i in gu_l:
                    return
                blk = blocks[i]
                slot = GUS[i % 2]
                w = blk["nfc"] * 128
                wg = slot.t[:, 0:DC * w].rearrange("p (c f) -> p c f", c=DC)
                wu = slot.t[:, 4096:4096 + DC * w].rearrange("p (c f) -> p c f", c=DC)
                k.dma(POOL, wg, blk["wg"], [], [slot.buf])
                k.dma(POOL, wu, blk["wu"], [], [slot.buf])
                gu_l[i] = (slot, wg, wu)

            def load_wd(i):
                if i >= nb or i in wd_l:
                    return
                blk = blocks[i]
                slot = WDS[i % 2]
                nfc = blk["nfc"]
                wd = slot.t[:, 0:nfc * D].rearrange("p (c f) -> p c f", c=nfc)
                k.dma(POOL, wd, blk["wd"], [], [slot.buf])
                wd_l[i] = (slot, wd)

            load_gu(0)
            load_gu(1)
            load_wd(0)
            load_wd(1)

            def stage1(u):
                i, tt = units[u]
                blk = blocks[i]
                slot, wg, wu = gu_l[i]
                nfc = blk["nfc"]
                G = gate_mul(blk) if gate_mul is not None else None
                cols = slice(tt * 512, (tt + 1) * 512)
                at = AT.get()
                for fc in range(nfc):
                    pg, pu = PSG.get(), PSG.get()
                    k.mm(pg.t[:], pg.buf, [(wg[:, kc, fc * 128:(fc + 1) * 128], HT[:, kc, cols],
                                            [slot.buf, HTb[kc][tt]]) for kc in range(DC)])
                    k.mm(pu.t[:], pu.buf, [(wu[:, kc, fc * 128:(fc + 1) * 128], HT[:, kc, cols],
                                            [slot.buf, HTb[kc][tt]]) for kc in range(DC)])
                    s_ = SS.get()
                    k.act(s_.t[:], pg.t[:], AF.Silu, [pg.buf], [s_.buf])
                    if G is None:
                        k.tt(DVE, at.t[:, fc, :], s_.t[:], pu.t[:], ALU.mult, [s_.buf, pu.buf], [at.buf])
                    else:
                        t = TT.get()
                        k.tt(DVE, t.t[:], G.t[:, cols], pu.t[:], ALU.mult, [G.buf, pu.buf], [t.buf])
                        k.tt(POOL, at.t[:, fc, :], s_.t[:], t.t[:], ALU.mult, [s_.buf, t.buf], [at.buf])
                return at

            def stage2(u, at):
                i, tt = units[u]
                blk = blocks[i]
                slot, wd = wd_l[i]
                for dm in range(DC):
                    py = PSY.get()
                    k.mm(py.t[:], py.buf, [(wd[:, fc, dm * 128:(dm + 1) * 128], at.t[:, fc, :],
                                            [slot.buf, at.buf]) for fc in range(blk["nfc"])])
                    add_to_x(dm, tt, py)

            at_next = stage1(0)
            for u in range(len(units)):
                at = at_next
                if u + 1 < len(units):
                    at_next = stage1(u + 1)
                    if units[u + 1][1] == NTT - 1:
                        load_gu(units[u + 1][0] + 2)
                stage2(u, at)
                i, tt = units[u]
                if tt == NTT - 1:
                    load_wd(i + 2)

        zeroed = [False]

        def dense_ffn_layer(l):
            j = l // 2
            with contextlib.ExitStack() as st:
                if not zeroed[0]:
                    zeroed[0] = True
                    ZB = Tile(sb("zb", [128, 5, D], BF16, st))
                    k.op(POOL, lambda: nc.gpsimd.memset(ZB.t[:], 0.0), [], [ZB.buf])
                    for e in range(NE):
                        k.dma(SP, HS_d[e * CAPR:e * CAPR + CAPM, :].rearrange("(c p) f -> p c f", p=128), ZB.t[:],
                              [ZB.buf], [HSb[0][0]])
                SQ = tmp_pool(st, "d_sq", 2, BF16)
                RT = tmp_pool(st, "d_rt", 1)
                RS = tmp_pool(st, "d_rs", 2)
                main_norm(l, 1, (SQ, RR(psum[6:8]), RT, RS))
                gv = w_fg[j].rearrange("(c p) f -> p c f", p=128)
                uv = w_fu[j].rearrange("(c p) f -> p c f", p=128)
                blocks = []
                f0 = 0
                while f0 < DFF:
                    w = min(512, DFF - f0)
                    blocks.append(dict(
                        wg=gv[:, :, f0:f0 + w], wu=uv[:, :, f0:f0 + w],
                        wd=w_fd[j, f0:f0 + w, :].rearrange("(c p) d -> p c d", p=128), nfc=w // 128))
                    f0 += w
                ffn_blocks(st, blocks)
                k.barrier()

        def moe_layer(l):
            j = l // 2
            with contextlib.ExitStack() as st:
                SQ = tmp_pool(st, "m_sq", 2, BF16)
                RT = tmp_pool(st, "m_rt", 1)
                RS = tmp_pool(st, "m_rs", 2)
                HF = tmp_pool(st, "m_hf", 3)
                RW = Tile(sb("m_rw", [128, DC, NE], F32, st))
                GATES = Tile(sb("m_gates", [128, TCH, NE], F32, st))
                GB = [Tile(sb(f"m_gb{i}", [128, S], F32, st)) for i in range(2)]
                GL = tmp_pool(st, "m_gl", 2, F32, (128, 128))
                SM = tmp_pool(st, "m_sm", 12, F32, (128, NE))
                SC = tmp_pool(st, "m_sc", 12, F32, (128, 1))
                k.dma(SP, RW.t[:], w_rt[j].rearrange("(c p) e -> p c e", p=128), [], [RW.buf])

                def hf_cb(tt, cols, rs, gbase):
                    lps = [psum[q] for q in range(4)]
                    for c in range(DC):
                        hf = HF.get()
                        k.stt(hf.t[:], XT[:, c, cols], gcol(gbase + c), rs.t[:], ALU.mult, ALU.mult,
                              [XTb[c][tt], GC.buf, rs.buf], [hf.buf])
                        k.cp(ACT, HT[:, c, cols], hf.t[:], [hf.buf], [HTb[c][tt]])
                        for q in range(4):
                            k.mm1(lps[q].t[:, 0:NE], lps[q].buf, hf.t[:, q * 128:(q + 1) * 128], RW.t[:, c, :],
                                  [hf.buf, RW.buf], c == 0, c == DC - 1)
                    for q in range(4):
                        tc = tt * 4 + q
                        lg = SM.get()
                        k.cp(DVE, lg.t[:], lps[q].t[:, 0:NE], [lps[q].buf], [lg.buf])
                        m1 = SC.get()
                        k.op(DVE, lambda: nc.vector.reduce_max(out=m1.t[:], in_=lg.t[:], axis=mybir.AxisListType.X),
                             [lg.buf], [m1.buf])
                        eq1 = SM.get()
                        k.ts(DVE, eq1.t[:], lg.t[:], m1.t[:], None, ALU.is_equal, None, [lg.buf, m1.buf], [eq1.buf])
                        l2 = SM.get()
                        k.stt(l2.t[:], eq1.t[:], -1e30, lg.t[:], ALU.mult, ALU.add, [eq1.buf, lg.buf], [l2.buf])
                        m2 = SC.get()
                        k.op(DVE, lambda: nc.vector.reduce_max(out=m2.t[:], in_=l2.t[:], axis=mybir.AxisListType.X),
                             [l2.buf], [m2.buf])
                        eq2 = SM.get()
                        k.ts(DVE, eq2.t[:], l2.t[:], m2.t[:], None, ALU.is_equal, None, [l2.buf, m2.buf], [eq2.buf])
                        dd = SC.get()
                        k.tt(DVE, dd.t[:], m2.t[:], m1.t[:], ALU.subtract, [m2.buf, m1.buf], [dd.buf])
                        e2 = SC.get()
                        k.act(e2.t[:], dd.t[:], AF.Exp, [dd.buf], [e2.buf])
                        den = SC.get()
                        k.ts(DVE, den.t[:], e2.t[:], 1.0, None, ALU.add, None, [e2.buf], [den.buf])
                        g1 = SC.get()
                        k.op(DVE, lambda: nc.vector.reciprocal(out=g1.t[:], in_=den.t[:]), [den.buf], [g1.buf])
                        g2 = SC.get()
                        k.tt(DVE, g2.t[:], e2.t[:], g1.t[:], ALU.mult, [e2.buf, g1.buf], [g2.buf])
                        ga = SM.get()
                        k.ts(DVE, ga.t[:], eq1.t[:], g1.t[:], None, ALU.mult, None, [eq1.buf, g1.buf], [ga.buf])
                        k.stt(GATES.t[:, tc, :], eq2.t[:], g2.t[:], ga.t[:], ALU.mult, ALU.add,
                              [eq2.buf, g2.buf, ga.buf], [GATES.buf])

                main_norm(l, 1, (SQ, RR(psum[6:8]), RT, RS), hf_cb)

                PSG2 = RR(psum[4:8])
                gstate = {"i": 0}

                def gate_mul(blk):
                    if blk["first"] and gstate.get("done") != blk["e"]:
                        gstate["done"] = blk["e"]
                        e = blk["e"]
                        G = GB[gstate["i"] % 2]
                        gstate["i"] += 1
                        for tt in range(NTT):
                            ps = PSG2.get()
                            for q in range(4):
                                tc = tt * 4 + q
                                gl = GL.get()
                                k.ts(DVE, gl.t[:], ONES.t[:], GATES.t[:, tc, e:e + 1], None, ALU.mult, None,
                                     [ONES.buf, GATES.buf], [gl.buf])
                                k.mm1(ps.t[:, q * 128:(q + 1) * 128], ps.buf, gl.t[:], ident, [gl.buf, CST.buf],
                                      True, True)
                            k.cp(ACT, G.t[:, tt * 512:(tt + 1) * 512], ps.t[:], [ps.buf], [G.buf])
                        gstate["G"] = G
                    return gstate["G"]

                blocks = []
                for e in range(NE):
                    gv = w_eg[j, e].rearrange("(c p) f -> p c f", p=128)
                    uv = w_eu[j, e].rearrange("(c p) f -> p c f", p=128)
                    for f0 in range(0, DFE, 512):
                        blocks.append(dict(
                            wg=gv[:, :, f0:f0 + 512], wu=uv[:, :, f0:f0 + 512],
                            wd=w_ed[j, e, f0:f0 + 512, :].rearrange("(c p) d -> p c d", p=128), nfc=4,
                            e=e, first=(f0 == 0)))
                ffn_blocks(st, blocks, gate_mul)
                k.barrier()


        HSb = [[Buf() for _ in range(2)] for _ in range(TCH)]
        CNTb = Buf()
        YSb = [Buf() for _ in range(NE)]
        HTM = HT[:].rearrange("p c s -> p (c s)").rearrange("p (t d) -> p t d", t=TCH)
        HTMb = [Buf() for _ in range(TCH)]
        LT = CST.t[:, 256:384]
        EBASE = CST.t[:, 384:512]
        EBASE2 = CST.t[:, 512:640]

        def moe_layer_sparse(l):
            j = l // 2
            with contextlib.ExitStack() as st:
                RW = Tile(sb("s_rw", [128, DC, NE], F32, st))
                EQ1 = Tile(sb("s_eq1", [128, TCH, NE], F32, st))
                EQ2 = Tile(sb("s_eq2", [128, TCH, NE], F32, st))
                G1 = Tile(sb("s_g1", [128, TCH], F32, st))
                G2 = Tile(sb("s_g2", [128, TCH], F32, st))
                PI1 = Tile(sb("s_pi1", [128, TCH], I32, st))
                PI2 = Tile(sb("s_pi2", [128, TCH], I32, st))
                PJ1 = Tile(sb("s_pj1", [128, TCH], I32, st))
                PJ2 = Tile(sb("s_pj2", [128, TCH], I32, st))
                G1E = Tile(sb("s_g1e", [128, TCH], F32, st))
                G2E = Tile(sb("s_g2e", [128, TCH], F32, st))
                GO = Tile(sb("s_go", [128, TCH, NE], F32, st))
                k.dma(SP, RW.t[:], w_rt[j].rearrange("(c p) e -> p c e", p=128), [], [RW.buf])
                GUS = [Tile(sb(f"s_gu{i}", [128, 8192], BF16, st)) for i in range(2)]
                WDS = [Tile(sb(f"s_wd{i}", [128, 4096], BF16, st)) for i in range(2)]
                PI1b = [Buf() for _ in range(NTT)]
                PI2b = [Buf() for _ in range(NTT)]
                blocks = []
                for e in range(NE):
                    gv = w_eg[j, e].rearrange("(c p) f -> p c f", p=128)
                    uv = w_eu[j, e].rearrange("(c p) f -> p c f", p=128)
                    for bi, f0 in enumerate(range(0, DFE, 512)):
                        blocks.append(dict(
                            wg=gv[:, :, f0:f0 + 512], wu=uv[:, :, f0:f0 + 512],
                            wd=w_ed[j, e, f0:f0 + 512, :].rearrange("(c p) d -> p c d", p=128), e=e, bi=bi))
                nb = len(blocks)
                gu_l, wd_l = {}, {}

                def load_gu(i):
                    if i >= nb or i in gu_l:
                        return
                    blk = blocks[i]
                    slot = GUS[i % 2]
                    wg = slot.t[:, 0:4096].rearrange("p (c f) -> p c f", c=DC)
                    wu = slot.t[:, 4096:8192].rearrange("p (c f) -> p c f", c=DC)
                    k.dma(POOL, wg, blk["wg"], [], [slot.buf])
                    k.dma(POOL, wu, blk["wu"], [], [slot.buf])
                    gu_l[i] = (slot, wg, wu)

                def load_wd(i):
                    if i >= nb or i in wd_l:
                        return
                    blk = blocks[i]
                    slot = WDS[i % 2]
                    wd = slot.t[:, 0:4096].rearrange("p (c f) -> p c f", c=4)
                    k.dma(POOL, wd, blk["wd"], [], [slot.buf])
                    wd_l[i] = (slot, wd)

                load_gu(0)
                load_gu(1)
                load_wd(0)
                load_wd(1)

                with contextlib.ExitStack() as st2:
                    SQ = tmp_pool(st2, "s_sq", 2, BF16)
                    RT = tmp_pool(st2, "s_rt", 1)
                    RS = tmp_pool(st2, "s_rs", 2)
                    HF = tmp_pool(st2, "s_hf", 3)
                    SM = tmp_pool(st2, "s_sm", 8, F32, (128, NE))
                    SC = tmp_pool(st2, "s_sc", 12, F32, (128, 1))
                    MS = Tile(sb("s_ms", [128, TCH * NE], F32, st2))
                    TOT = Tile(sb("s_tot", [128, TCH, NE], F32, st2))
                    CUM = Tile(sb("s_cum", [128, TCH, NE], F32, st2))
                    PB = Tile(sb("s_pb", [128, TCH, NE], F32, st2))
                    PM = Tile(sb("s_pm", [128, TCH, NE], F32, st2))
                    PB2 = Tile(sb("s_pb2", [128, TCH, NE], F32, st2))
                    PF = Tile(sb("s_pf", [128, TCH], F32, st2))
                    OV = Tile(sb("s_ov", [128, TCH, NE], F32, st2))
                    KP = Tile(sb("s_kp", [128, TCH], F32, st2))
                    NEC = Tile(sb("s_nec", [128, NE], F32, st2))
                    MXF = Tile(sb("s_mxf", [128, 1], F32, st2))
                    MXI = Tile(sb("s_mxi", [128, 1], I32, st2))
                    PTR = RR(psum[4:6])
                    LG = tmp_pool(st2, "s_lg", 8, F32, (128, NE))
                    pending = []

                    def hf_cb(tt, cols, rs, gbase):
                        lps = [psum[q] for q in range(4)]
                        for c in range(DC):
                            hf = HF.get()
                            k.stt(hf.t[:], XT[:, c, cols], gcol(gbase + c), rs.t[:], ALU.mult, ALU.mult,
                                  [XTb[c][tt], GC.buf, rs.buf], [hf.buf])
                            for q in range(4):
                                k.mm1(lps[q].t[:, 0:NE], lps[q].buf, hf.t[:, q * 128:(q + 1) * 128], RW.t[:, c, :],
                                      [hf.buf, RW.buf], c == 0, c == DC - 1)
                            ptr = PTR.get()
                            for q in range(4):
                                k.transpose(ptr.t[:, q * 128:(q + 1) * 128], ptr.buf, hf.t[:, q * 128:(q + 1) * 128],
                                            ident, [hf.buf, CST.buf])
                            k.cp(ACT, HTM[:, tt * 4:(tt + 1) * 4, c * 128:(c + 1) * 128],
                                 ptr.t[:].rearrange("p (q d) -> p q d", q=4), [ptr.buf],
                                 [HTMb[tt * 4 + q] for q in range(4)])
                        lgs = []
                        for q in range(4):
                            lg = LG.get()
                            k.cp(DVE, lg.t[:], lps[q].t[:, 0:NE], [lps[q].buf], [lg.buf])
                            lgs.append(lg)
                        if pending:
                            pending.pop()()
                        pending.append(lambda: route(tt, lgs))

                    def route(tt, lgs):
                        for q in range(4):
                            tc = tt * 4 + q
                            lg = lgs[q]
                            m1 = SC.get()
                            k.op(DVE, lambda: nc.vector.reduce_max(out=m1.t[:], in_=lg.t[:], axis=mybir.AxisListType.X),
                                 [lg.buf], [m1.buf])
                            k.ts(DVE, EQ1.t[:, tc, :], lg.t[:], m1.t[:], None, ALU.is_equal, None,
                                 [lg.buf, m1.buf], [EQ1.buf])
                            l2 = SM.get()
                            k.stt(l2.t[:], EQ1.t[:, tc, :], -1e30, lg.t[:], ALU.mult, ALU.add, [EQ1.buf, lg.buf], [l2.buf])
                            m2 = SC.get()
                            k.op(DVE, lambda: nc.vector.reduce_max(out=m2.t[:], in_=l2.t[:], axis=mybir.AxisListType.X),
                                 [l2.buf], [m2.buf])
                            k.ts(DVE, EQ2.t[:, tc, :], l2.t[:], m2.t[:], None, ALU.is_equal, None,
                                 [l2.buf, m2.buf], [EQ2.buf])
                            dd = SC.get()
                            k.tt(DVE, dd.t[:], m2.t[:], m1.t[:], ALU.subtract, [m2.buf, m1.buf], [dd.buf])
                            e2 = SC.get()
                            k.act(e2.t[:], dd.t[:], AF.Exp, [dd.buf], [e2.buf])
                            den = SC.get()
                            k.ts(DVE, den.t[:], e2.t[:], 1.0, None, ALU.add, None, [e2.buf], [den.buf])
                            k.op(DVE, lambda: nc.vector.reciprocal(out=G1.t[:, tc:tc + 1], in_=den.t[:]),
                                 [den.buf], [G1.buf])
                            k.tt(DVE, G2.t[:, tc:tc + 1], e2.t[:], G1.t[:, tc:tc + 1], ALU.mult, [e2.buf, G1.buf], [G2.buf])
                        ranks(tt)

                    def ranks(tt):
                        fs = slice(32 * tt, 32 * tt + 32)
                        ts_ = slice(4 * tt, 4 * tt + 4)
                        eq1f = EQ1.t[:].rearrange("p t e -> p (t e)")
                        eq2f = EQ2.t[:].rearrange("p t e -> p (t e)")
                        k.tt(DVE, MS.t[:, fs], eq1f[:, fs], eq2f[:, fs], ALU.add, [EQ1.buf, EQ2.buf], [MS.buf])
                        pe_, pt_ = PTR.get(), PTR.get()
                        k.mm1(pe_.t[:, 0:32], pe_.buf, LT, MS.t[:, fs], [CST.buf, MS.buf], True, True)
                        k.mm1(pt_.t[:, 0:32], pt_.buf, ONES.t[:], MS.t[:, fs], [ONES.buf, MS.buf], True, True)
                        k.cp(DVE, TOT.t[:].rearrange("p t e -> p (t e)")[:, fs], pt_.t[:, 0:32], [pt_.buf], [TOT.buf])
                        if tt == 0:
                            k.op(DVE, lambda: nc.vector.memset(CUM.t[:, 0, :], 0.0), [], [CUM.buf])
                        for tc in range(4 * tt + 1, min(4 * tt + 5, TCH)):
                            k.tt(DVE, CUM.t[:, tc, :], CUM.t[:, tc - 1, :], TOT.t[:, tc - 1, :], ALU.add,
                                 [CUM.buf, TOT.buf], [CUM.buf])
                        pbf = PB.t[:].rearrange("p t e -> p (t e)")[:, fs]
                        k.tt(DVE, pbf, CUM.t[:].rearrange("p t e -> p (t e)")[:, fs], pe_.t[:, 0:32], ALU.add,
                             [CUM.buf, pe_.buf], [PB.buf])
                        ovf = OV.t[:].rearrange("p t e -> p (t e)")[:, fs]
                        k.ts(DVE, ovf, pbf, float(OVT) - 0.5, None, ALU.is_gt, None, [PB.buf], [OV.buf])
                        pb2 = PB2.t[:].rearrange("p t e -> p (t e)")[:, fs]
                        k.tt(DVE, pb2, pbf, EBASE2[:, fs], ALU.add, [PB.buf, CST.buf], [PB2.buf])
                        k.tt(DVE, pbf, pbf, EBASE[:, fs], ALU.add, [PB.buf, CST.buf], [PB.buf])
                        pmf = PM.t[:].rearrange("p t e -> p (t e)")[:, fs]
                        for EQ, PI, PIb, PJ, G, GE in ((EQ1, PI1, PI1b, PJ1, G1, G1E), (EQ2, PI2, PI2b, PJ2, G2, G2E)):
                            eqf = EQ.t[:].rearrange("p t e -> p (t e)")[:, fs]
                            k.tt(DVE, pmf, eqf, pbf, ALU.mult, [EQ.buf, PB.buf], [PM.buf])
                            k.op(DVE, lambda: nc.vector.reduce_sum(out=PF.t[:, ts_], in_=PM.t[:, ts_, :],
                                                                   axis=mybir.AxisListType.X), [PM.buf], [PF.buf])
                            k.cp(DVE, PI.t[:, ts_], PF.t[:, ts_], [PF.buf], [PIb[tt]])
                            k.tt(DVE, pmf, eqf, ovf, ALU.mult, [EQ.buf, OV.buf], [PM.buf])
                            k.op(DVE, lambda: nc.vector.reduce_sum(out=KP.t[:, ts_], in_=PM.t[:, ts_, :],
                                                                   axis=mybir.AxisListType.X), [PM.buf], [KP.buf])
                            k.ts(DVE, KP.t[:, ts_], KP.t[:, ts_], -1.0, 1.0, ALU.mult, ALU.add, [KP.buf], [KP.buf])
                            k.tt(DVE, pmf, eqf, pb2, ALU.mult, [EQ.buf, PB2.buf], [PM.buf])
                            k.op(DVE, lambda: nc.vector.reduce_sum(out=PF.t[:, ts_], in_=PM.t[:, ts_, :],
                                                                   axis=mybir.AxisListType.X), [PM.buf], [PF.buf])
                            k.tt(DVE, PF.t[:, ts_], PF.t[:, ts_], KP.t[:, ts_], ALU.mult, [PF.buf, KP.buf], [PF.buf])
                            k.cp(DVE, PJ.t[:, ts_], PF.t[:, ts_], [PF.buf], [PJ.buf])
                            k.tt(DVE, GE.t[:, ts_], G.t[:, ts_], KP.t[:, ts_], ALU.mult, [G.buf, KP.buf], [GE.buf])
                        for tc in range(4 * tt, 4 * tt + 4):
                            k.ts(DVE, GO.t[:, tc, :], EQ1.t[:, tc, :], G1.t[:, tc:tc + 1], None, ALU.mult, None,
                                 [EQ1.buf, G1.buf], [GO.buf])
                            k.stt(GO.t[:, tc, :], EQ2.t[:, tc, :], G2.t[:, tc:tc + 1], GO.t[:, tc, :], ALU.mult, ALU.add,
                                  [EQ2.buf, G2.buf, GO.buf], [GO.buf])
                        gof = GO.t[:].rearrange("p t e -> p (t e)")[:, fs]
                        k.tt(DVE, gof, gof, ovf, ALU.mult, [GO.buf, OV.buf], [GO.buf])
                        for tc in range(4 * tt, 4 * tt + 4):
                            for kk, (PI, PIb) in enumerate(((PI1, PI1b), (PI2, PI2b))):
                                k.idma(HS_d[:, :], bass.IndirectOffsetOnAxis(ap=PI.t[:, tc:tc + 1], axis=0),
                                       HTM[:, tc, :], None, [HTMb[tc], PIb[tt]], [HSb[tc][kk]])
                        if tt == NTT - 1:
                            k.tt(DVE, NEC.t[:], CUM.t[:, TCH - 1, :], TOT.t[:, TCH - 1, :], ALU.add,
                                 [CUM.buf, TOT.buf], [NEC.buf])
                            k.op(DVE, lambda: nc.vector.reduce_max(out=MXF.t[:], in_=NEC.t[:], axis=mybir.AxisListType.X),
                                 [NEC.buf], [MXF.buf])
                            k.cp(DVE, MXI.t[:], MXF.t[:], [MXF.buf], [MXI.buf])
                            k.dma(SP, CNT_d[0:1, 0:1], MXI.t[0:1, 0:1], [MXI.buf], [CNTb])

                    main_norm(l, 1, (SQ, RR(psum[6:8]), RT, RS), hf_cb)
                    pending.pop()()
                    k.barrier()

                with contextlib.ExitStack() as st3:
                    HSE = Tile(sb("s_hse", [128, 5, D], BF16, st3))
                    HET = Tile(sb("s_het", [128, DC, CAPM], BF16, st3))
                    AT = tmp_pool(st3, "s_at", 2, BF16, (128, 4, CAPM))
                    SS = tmp_pool(st3, "s_ss", 2, F32, (128, CAPM))
                    YACC = Tile(sb("s_yacc", [128, 5, D], F32, st3))
                    PSG = RR(psum[0:6])
                    PSY = RR(psum[6:8])
                    all_hs = [HSb[tc][kk] for tc in range(TCH) for kk in range(2)]

                    def prep(e):
                        k.dma(SP, HSE.t[:], HS_d[e * CAPR:e * CAPR + CAPM, :].rearrange("(c p) f -> p c f", p=128),
                              all_hs, [HSE.buf])
                        for dc in range(DC):
                            ps = PSY.get()
                            psb = ps.t[:].bitcast(BF16)
                            for sc in range(5):
                                k.transpose(psb[:, sc * 128:(sc + 1) * 128], ps.buf, HSE.t[:, sc, dc * 128:(dc + 1) * 128],
                                            IDB.t[:], [HSE.buf, IDB.buf])
                            k.cp(ACT if dc % 2 == 0 else DVE, HET.t[:, dc, :], psb[:, 0:CAPM], [ps.buf], [HET.buf])

                    SA = 384
                    SB = CAPM - SA

                    def stage1(u):
                        slot, wg, wu = gu_l[u]
                        at = AT.get()
                        for fc in range(4):
                            pgA, puA, pB = PSG.get(), PSG.get(), PSG.get()
                            fs = slice(fc * 128, (fc + 1) * 128)
                            k.mm(pgA.t[:, 0:SA], pgA.buf, [(wg[:, kc, fs], HET.t[:, kc, 0:SA], [slot.buf, HET.buf])
                                                           for kc in range(DC)])
                            k.mm(puA.t[:, 0:SA], puA.buf, [(wu[:, kc, fs], HET.t[:, kc, 0:SA], [slot.buf, HET.buf])
                                                           for kc in range(DC)])
                            k.mm(pB.t[:, 0:SB], pB.buf, [(wg[:, kc, fs], HET.t[:, kc, SA:CAPM], [slot.buf, HET.buf])
                                                         for kc in range(DC)])
                            k.mm(pB.t[:, SB:2 * SB], pB.buf, [(wu[:, kc, fs], HET.t[:, kc, SA:CAPM], [slot.buf, HET.buf])
                                                              for kc in range(DC)])
                            s_ = SS.get()
                            k.act(s_.t[:, 0:SA], pgA.t[:, 0:SA], AF.Silu, [pgA.buf], [s_.buf])
                            k.act(s_.t[:, SA:CAPM], pB.t[:, 0:SB], AF.Silu, [pB.buf], [s_.buf])
                            k.tt(DVE, at.t[:, fc, 0:SA], s_.t[:, 0:SA], puA.t[:, 0:SA], ALU.mult, [s_.buf, puA.buf], [at.buf])
                            k.tt(DVE, at.t[:, fc, SA:CAPM], s_.t[:, SA:CAPM], pB.t[:, SB:2 * SB], ALU.mult,
                                 [s_.buf, pB.buf], [at.buf])
                        return at

                    def stage2(u, at):
                        slot, wd = wd_l[u]
                        blk = blocks[u]
                        for sc in range(5):
                            for hf_ in range(2):
                                py = PSY.get()
                                k.mm(py.t[:], py.buf, [(at.t[:, fc, sc * 128:(sc + 1) * 128],
                                                        wd[:, fc, hf_ * 512:(hf_ + 1) * 512], [at.buf, slot.buf])
                                                       for fc in range(4)])
                                ya = YACC.t[:, sc, hf_ * 512:(hf_ + 1) * 512]
                                if blk["bi"] == 0:
                                    k.cp(ACT, ya, py.t[:], [py.buf], [YACC.buf])
                                else:
                                    k.tt(DVE, ya, ya, py.t[:], ALU.add, [YACC.buf, py.buf], [YACC.buf])
                        if blk["bi"] == 3:
                            e = blk["e"]
                            k.dma(SP, YS_d[e * CAPM:(e + 1) * CAPM, :].rearrange("(c p) f -> p c f", p=128),
                                  YACC.t[:], [YACC.buf], [YSb[e]])

                    prep(0)
                    at_next = stage1(0)
                    load_gu(2)
                    for u in range(nb):
                        at = at_next
                        nxt_new_expert = (u + 1 < nb) and blocks[u + 1]["bi"] == 0
                        if nxt_new_expert:
                            prep(blocks[u + 1]["e"])
                            stage2(u, at)
                            load_wd(u + 2)
                            at_next = stage1(u + 1)
                            load_gu(u + 3)
                        else:
                            if u + 1 < nb:
                                at_next = stage1(u + 1)
                                load_gu(u + 3)
                            stage2(u, at)
                            load_wd(u + 2)
                    k.barrier()

                def fallback():
                    with contextlib.ExitStack() as sto:
                        SQ = tmp_pool(sto, "o_sq", 2, BF16)
                        RT = tmp_pool(sto, "o_rt", 1)
                        RS = tmp_pool(sto, "o_rs", 2)
                        GB = [Tile(sb(f"o_gb{i}", [128, S], F32, sto)) for i in range(2)]
                        GL = tmp_pool(sto, "o_gl", 2, F32, (128, 128))
                        main_norm(l, 1, (SQ, RR(psum[6:8]), RT, RS))
                        PSG2 = RR(psum[4:8])
                        gstate = {"i": 0}

                        def gate_mul(blk):
                            if blk["first"] and gstate.get("done") != blk["e"]:
                                gstate["done"] = blk["e"]
                                e = blk["e"]
                                G = GB[gstate["i"] % 2]
                                gstate["i"] += 1
                                for tt in range(NTT):
                                    ps = PSG2.get()
                                    for q in range(4):
                                        tc = tt * 4 + q
                                        gl = GL.get()
                                        k.ts(DVE, gl.t[:], ONES.t[:], GO.t[:, tc, e:e + 1], None, ALU.mult, None,
                                             [ONES.buf, GO.buf], [gl.buf])
                                        k.mm1(ps.t[:, q * 128:(q + 1) * 128], ps.buf, gl.t[:], ident, [gl.buf, CST.buf],
                                              True, True)
                                    k.cp(ACT, G.t[:, tt * 512:(tt + 1) * 512], ps.t[:], [ps.buf], [G.buf])
                                gstate["G"] = G
                            return gstate["G"]

                        blocks = []
                        for e in range(NE):
                            gv = w_eg[j, e].rearrange("(c p) f -> p c f", p=128)
                            uv = w_eu[j, e].rearrange("(c p) f -> p c f", p=128)
                            for f0 in range(0, DFE, 512):
                                blocks.append(dict(
                                    wg=gv[:, :, f0:f0 + 512], wu=uv[:, :, f0:f0 + 512],
                                    wd=w_ed[j, e, f0:f0 + 512, :].rearrange("(c p) d -> p c d", p=128), nfc=4,
                                    e=e, first=(f0 == 0)))
                        ffn_blocks(sto, blocks, gate_mul, slots=(GUS, WDS))
                        k.barrier()

                regs = nc.alloc_registers(f"cnt_{l}_{uid[0]}")
                for reg in regs:
                    E = {mybir.EngineType.PE: PE, mybir.EngineType.DVE: DVE, mybir.EngineType.Activation: ACT,
                         mybir.EngineType.Pool: POOL, mybir.EngineType.SP: SP}[reg.engine]
                    k.wait(E, CNTb.w)
                    nc.reg_load(reg, CNT_d[0:1, 0:1])
                k.guarded(regs, OVT, fallback)

                with contextlib.ExitStack() as st4:
                    GA = tmp_pool(st4, "s_ga", 2, F32, (128, D))
                    GB_ = tmp_pool(st4, "s_gb", 2, F32, (128, D))
                    CB = tmp_pool(st4, "s_cb", 2, F32, (128, D))
                    PSC = RR(psum)
                    for tc in range(TCH):
                        tt = tc // 4
                        ga, gb = GA.get(), GB_.get()
                        k.idma(ga.t[:, :], None, YS_d[:, :], bass.IndirectOffsetOnAxis(ap=PJ1.t[:, tc:tc + 1], axis=0),
                               YSb + [PJ1.buf], [ga.buf])
                        k.idma(gb.t[:, :], None, YS_d[:, :], bass.IndirectOffsetOnAxis(ap=PJ2.t[:, tc:tc + 1], axis=0),
                               YSb + [PJ2.buf], [gb.buf])
                        cb = CB.get()
                        k.ts(DVE, cb.t[:], ga.t[:], G1E.t[:, tc:tc + 1], None, ALU.mult, None, [ga.buf, G1E.buf], [cb.buf])
                        k.stt(cb.t[:], gb.t[:], G2E.t[:, tc:tc + 1], cb.t[:], ALU.mult, ALU.add,
                              [gb.buf, G2E.buf, cb.buf], [cb.buf])
                        for h_ in range(2):
                            ps = PSC.get()
                            for q in range(4):
                                dc = h_ * 4 + q
                                k.transpose(ps.t[:, q * 128:(q + 1) * 128], ps.buf, cb.t[:, dc * 128:(dc + 1) * 128],
                                            ident, [cb.buf, CST.buf])
                            xv = XT[:, h_ * 4:(h_ + 1) * 4, tc * 128:(tc + 1) * 128]
                            k.tt(DVE, xv, xv, ps.t[:].rearrange("p (q d) -> p q d", q=4), ALU.add,
                                 [XTb[h_ * 4 + q][tt] for q in range(4)] + [ps.buf],
                                 [XTb[h_ * 4 + q][tt] for q in range(4)])
                k.barrier()

        def mla_layer(l, b):
            j = l // 2
            with contextlib.ExitStack() as st:
                W1 = Tile(sb("a_w1", [128, DC, D], BF16, st))
                HWP = tmp_pool(st, "a_hw", 2, BF16, (128, 1088))
                CQ = Tile(sb("a_cq", [128, 3, S], BF16, st))
                CKV = Tile(sb("a_ckv", [128, 2, S], BF16, st))
                T1 = Tile(sb("a_t1", [128, S], F32, st))
                T2 = Tile(sb("a_t2", [128, S], F32, st))
                QN = Tile(sb("a_qn", [128, S], BF16, st))
                QR = Tile(sb("a_qr", [128, S], BF16, st))
                KN = Tile(sb("a_kn", [128, S], BF16, st))
                KR = Tile(sb("a_kr", [128, S], BF16, st))
                VV = Tile(sb("a_v", [128, TCH, 128], BF16, st))
                SQ = tmp_pool(st, "a_sq", 2, BF16)
                RT = tmp_pool(st, "a_rt", 1)
                RS = tmp_pool(st, "a_rs", 2)
                TA = tmp_pool(st, "a_ta", 6)
                PT = tmp_pool(st, "a_pt", 5, BF16, (128, 512))
                PI = Tile(sb("a_pi", [64, 512], I32, st))
                KI = Tile(sb("a_ki", [64, 512], I32, st))

                wd_v = w_mdown[j].rearrange("(c p) f -> p c f", p=128)
                for c in range(DC):
                    k.dma(POOL, W1.t[:, c, 0:704], wd_v[:, c, :], [], [W1.buf])
                wuq_v = w_muq[j].rearrange("(c p) f -> p c f", p=128)
                wukv_v = w_mukv[j].rearrange("(c p) f -> p c f", p=128)

                def load_head(hd):
                    hw = HWP.get()
                    k.dma(POOL, hw.t[:, 0:576].rearrange("p (c f) -> p c f", c=3),
                          wuq_v[:, :, hd * 192:(hd + 1) * 192], [], [hw.buf])
                    k.dma(POOL, hw.t[:, 576:1088].rearrange("p (c f) -> p c f", c=2),
                          wukv_v[:, :, hd * 256:(hd + 1) * 256], [], [hw.buf])
                    return hw

                hws = {0: load_head(0), 1: load_head(1)}
                ZPAD = Buf()
                k.op(POOL, lambda: nc.gpsimd.memset(QR.t[64:128, :], 0.0), [], [ZPAD])
                k.op(POOL, lambda: nc.gpsimd.memset(KR.t[64:128, :], 0.0), [], [ZPAD])

                PIS = [PI, KI]
                T2b, T1cb = Buf(), Buf()

                PTMP = RR([Tile.__new__(Tile) for _ in range(4)])
                for i_, tl in enumerate(PTMP.items):
                    src_t = QN if i_ < 2 else KN
                    tl.t = src_t.t[:, (i_ % 2) * 1024:(i_ % 2 + 1) * 1024].bitcast(F32)
                    tl.buf = Buf()

                def reduce_angle(E, a, abuf):
                    TP = TA if E is DVE else PTMP

                    def fma(x, c):
                        if E is DVE:
                            k.stt(a, x, c, a, ALU.mult, ALU.add, [kf.buf, abuf], [abuf])
                        else:
                            tm = TP.get()
                            k.ts(E, tm.t[0:64, :], x, c, None, ALU.mult, None, [kf.buf], [tm.buf])
                            k.tt(E, a, tm.t[0:64, :], a, ALU.add, [tm.buf, abuf], [abuf])

                    ki = TP.get()
                    ki_i = ki.t[0:64, :].bitcast(I32)
                    k.ts(E, ki_i, a, float(1.0 / (2 * np.pi)), None, ALU.mult, None, [abuf], [ki.buf])
                    kf = TP.get()
                    k.cp(E, kf.t[0:64, :], ki_i, [ki.buf], [kf.buf])
                    fma(kf.t[0:64, :], -6.28125)
                    fma(kf.t[0:64, :], -0.0019353071795864769)
                    k.ts(E, kf.t[0:64, :], a, float(np.pi), None, ALU.is_gt, None, [abuf], [kf.buf])
                    fma(kf.t[0:64, :], float(-2 * np.pi))
                    k.ts(E, kf.t[0:64, :], a, float(-np.pi), None, ALU.is_lt, None, [abuf], [kf.buf])
                    fma(kf.t[0:64, :], float(2 * np.pi))
                    k.ts(E, a, a, -3.1415925, 3.1415925, ALU.max, ALU.min, [abuf], [abuf])

                def tables(tt):
                    cols = slice(tt * 512, (tt + 1) * 512)
                    pi_t = PIS[tt % 2]
                    k.dma(SP, pi_t.t[:], pos_d[b:b + 1, cols].partition_broadcast(64), [], [pi_t.buf])
                    pf = TA.get()
                    k.cp(DVE, pf.t[0:64, :], pi_t.t[:], [pi_t.buf], [pf.buf])
                    k.ts(DVE, T2.t[0:64, cols], pf.t[0:64, :], gcol(GC_FREQ, 0, 64), None, ALU.mult, None,
                         [pf.buf, GC.buf], [T2b])
                    k.ts(DVE, T1.t[0:64, cols], pf.t[0:64, :], gcol(GC_FREQ, 0, 64), float(np.pi / 2),
                         ALU.mult, ALU.add, [pf.buf, GC.buf], [T1cb])
                    reduce_angle(DVE, T1.t[0:64, cols], T1cb)
                    reduce_angle(DVE, T2.t[0:64, cols], T2b)

                main_norm(l, 0, (SQ, RR(psum[6:8]), RT, RS))

                PSA = RR(psum[0:6])
                PSS = RR(psum[6:8])
                for tt in range(NTT):
                    cols = slice(tt * 512, (tt + 1) * 512)

                    def down(m0, m1):
                        ps = PSA.get()
                        k.mm(ps.t[0:m1 - m0, :], ps.buf, [(W1.t[:, kc, m0:m1], HT[:, kc, cols], [W1.buf, HTb[kc][tt]])
                                                          for kc in range(DC)])
                        return ps
                    dq = [down(c * 128, (c + 1) * 128) for c in range(3)]
                    rs = rms_stats((SQ, PSS, RT, RS), [(p.t[:], p.buf, 0, 128) for p in dq], QL)
                    for c in range(3):
                        k.stt(CQ.t[:, c, cols], dq[c].t[:], gcol(GC_QA + j * 3 + c), rs.t[:], ALU.mult, ALU.mult,
                              [dq[c].buf, GC.buf, rs.buf], [CQ.buf])
                    dkv = [down(QL + c * 128, QL + (c + 1) * 128) for c in range(2)]
                    pkr = down(QL + KVL, QL + KVL + ROPE)
                    rs = rms_stats((SQ, PSS, RT, RS), [(p.t[:], p.buf, 0, 128) for p in dkv], KVL)
                    for c in range(2):
                        k.stt(CKV.t[:, c, cols], dkv[c].t[:], gcol(GC_KVA + j * 2 + c), rs.t[:], ALU.mult, ALU.mult,
                              [dkv[c].buf, GC.buf, rs.buf], [CKV.buf])
                    k.cp(DVE, T1.t[64:128, cols], pkr.t[0:64, :], [pkr.buf], [T1.buf])
                    tables(tt)
                for tt in range(NTT):
                    cols = slice(tt * 512, (tt + 1) * 512)
                    k.act(T1.t[0:64, cols], T1.t[0:64, cols], AF.Sin, [T1cb], [T1cb])
                    k.act(T2.t[0:64, cols], T2.t[0:64, cols], AF.Sin, [T2b], [T2b])
                    k.ts(DVE, T2.t[0:64, cols], T2.t[0:64, cols], gcol(GC_SIGN, 0, 64), None, ALU.mult, None,
                         [T2b, GC.buf], [T2b])

                wo_v = w_mo[j].rearrange("(c p) f -> p c f", p=128)
                for c in range(DC):
                    k.dma(POOL, W1.t[:, c, :], wo_v[:, c, :], [], [W1.buf])

                QNb = [Buf() for _ in range(NTT)]
                QRb = [Buf() for _ in range(NTT)]
                KNb = [Buf() for _ in range(NTT)]
                KRb = [Buf() for _ in range(NTT)]
                VVb = [Buf() for _ in range(NTT)]

                def rope(E, src, dst, dbuf, cols):
                    ra = TA.get()
                    k.tt(E, ra.t[0:64, :], src.t[0:64, :], T1.t[0:64, cols], ALU.mult, [src.buf, T1cb], [ra.buf])
                    rb = TA.get()
                    k.tt(E, rb.t[0:32, :], src.t[32:64, :], T2.t[32:64, cols], ALU.mult, [src.buf, T2b], [rb.buf])
                    k.tt(E, rb.t[32:64, :], src.t[0:32, :], T2.t[0:32, cols], ALU.mult, [src.buf, T2b], [rb.buf])
                    k.tt(E, dst.t[0:64, cols], ra.t[0:64, :], rb.t[0:64, :], ALU.add, [ra.buf, rb.buf], [dbuf])

                PSP = RR(psum[0:4])
                PSS2 = RR(psum[4:6])
                gqn, gqr = GC_QN + j * 2, GC_QN + j * 2 + 1
                gkn, gkr = GC_KN + j * 2, GC_KN + j * 2 + 1

                def proj(hw, tt):
                    WUQ = hw.t[:, 0:576].rearrange("p (c f) -> p c f", c=3)
                    WUKV = hw.t[:, 576:1088].rearrange("p (c f) -> p c f", c=2)
                    cols = slice(tt * 512, (tt + 1) * 512)
                    pa, pb_, pc = PSP.get(), PSP.get(), PSP.get()
                    k.mm(pa.t[:], pa.buf, [(WUQ[:, kc, 0:128], CQ.t[:, kc, cols], [hw.buf, CQ.buf])
                                           for kc in range(3)])
                    k.mm(pb_.t[0:64, :], pb_.buf, [(WUQ[:, kc, 128:192], CQ.t[:, kc, cols], [hw.buf, CQ.buf])
                                                    for kc in range(3)])
                    k.mm(pc.t[:], pc.buf, [(WUKV[:, kc, 0:128], CKV.t[:, kc, cols], [hw.buf, CKV.buf])
                                           for kc in range(2)])
                    pv = PSS2.get()
                    for q in range(4):
                        tc = tt * 4 + q
                        k.mm(pv.t[:, q * 128:(q + 1) * 128], pv.buf,
                             [(CKV.t[:, kc, tc * 128:(tc + 1) * 128], WUKV[:, kc, 128:256], [CKV.buf, hw.buf])
                              for kc in range(2)])
                    k.cp(ACT, VV.t[:, tt * 4:(tt + 1) * 4, :], pv.t[:].rearrange("p (q d) -> p q d", q=4),
                         [pv.buf], [VVb[tt]])
                    rs = rms_stats((SQ, PSS2, RT, RS), [(pa.t[:], pa.buf, 0, 128), (pb_.t[0:64, :], pb_.buf, 0, 64)],
                                   192)
                    k.stt(QN.t[:, cols], pa.t[:], gcol(gqn), rs.t[:], ALU.mult, ALU.mult,
                          [pa.buf, GC.buf, rs.buf, T1cb, T2b], [QNb[tt]])
                    tq = TA.get()
                    k.stt(tq.t[0:64, :], pb_.t[0:64, :], gcol(gqr, 0, 64), rs.t[0:64, :], ALU.mult, ALU.mult,
                          [pb_.buf, GC.buf, rs.buf], [tq.buf])
                    rope(DVE, tq, QR, QRb[tt], cols)
                    rs = rms_stats((SQ, PSS2, RT, RS), [(pc.t[:], pc.buf, 0, 128),
                                                        (T1.t[64:128, cols], T1.buf, 64, 128)], 192)
                    k.stt(KN.t[:, cols], pc.t[:], gcol(gkn), rs.t[:], ALU.mult, ALU.mult,
                          [pc.buf, GC.buf, rs.buf, T1cb, T2b], [KNb[tt]])
                    tk = TA.get()
                    k.stt(tk.t[0:64, :], T1.t[64:128, cols], gcol(gkr, 64, 128), rs.t[64:128, :],
                          ALU.mult, ALU.mult, [T1.buf, GC.buf, rs.buf], [tk.buf])
                    rope(POOL, tk, KR, KRb[tt], cols)

                def attn(hd, jt):
                    qcols0 = jt * 512
                    po, pl = (psum[4], psum[5]) if jt % 2 == 0 else (psum[6], psum[7])
                    nck = 4 * (jt + 1)

                    def score(c):
                        i = c - 4 * jt
                        q_lo = 128 * i if i > 0 else 0
                        n = 512 - q_lo
                        qs = slice(qcols0 + q_lo, qcols0 + 512)
                        ks = slice(c * 128, (c + 1) * 128)
                        kt = c // 4
                        pS = PSP.get()
                        k.mm(pS.t[:, 0:n], pS.buf, [(KN.t[:, ks], QN.t[:, qs], [KNb[kt], QNb[jt]]),
                                                    (KR.t[:, ks], QR.t[:, qs], [KRb[kt], QRb[jt], ZPAD])])
                        pt = PT.get()
                        k.act(pt.t[:, 0:n], pS.t[:, 0:n], AF.Exp, [pS.buf], [pt.buf])
                        if i >= 0:
                            k.tt(DVE, pt.t[:, 0:128], pt.t[:, 0:128], TRI.t[:], ALU.mult, [pt.buf, TRI.buf], [pt.buf])
                        return pt, q_lo, n

                    LOOK = 3
                    pend = [score(c) for c in range(min(LOOK, nck))]
                    for c in range(nck):
                        pt, q_lo, n = pend.pop(0)
                        if c + LOOK < nck:
                            pend.append(score(c + LOOK))
                        k.mm1(po.t[:, q_lo:512], po.buf, VV.t[:, c, :], pt.t[:, 0:n], [VVb[c // 4], pt.buf],
                              c == 0, c == nck - 1)
                        k.mm1(pl.t[:, q_lo:512], pl.buf, ONES_B.t[:], pt.t[:, 0:n], [ONES_B.buf, pt.buf],
                              c == 0, c == nck - 1)
                    rl = TA.get()
                    k.act(rl.t[:], pl.t[:], AF.Ln, [pl.buf], [rl.buf])
                    k.act(rl.t[:], rl.t[:], AF.Exp, [rl.buf], [rl.buf], scale=-1.0)
                    k.tt(DVE, HT[:, hd, qcols0:qcols0 + 512], po.t[:], rl.t[:], ALU.mult, [po.buf, rl.buf],
                         [HTb[hd][jt]])

                for hd in range(NH):
                    if hd + 1 < NH and (hd + 1) not in hws:
                        hws[hd + 1] = load_head(hd + 1)
                    hw = hws.pop(hd)
                    proj(hw, 0)
                    for tt in range(1, NTT):
                        proj(hw, tt)
                        attn(hd, tt - 1)
                    attn(hd, NTT - 1)
                PSY = RR(psum[0:4])
                for tt in range(NTT):
                    cols = slice(tt * 512, (tt + 1) * 512)
                    for dm in range(DC):
                        py = PSY.get()
                        k.mm(py.t[:], py.buf, [(W1.t[:, hc, dm * 128:(dm + 1) * 128], HT[:, hc, cols],
                                                [W1.buf, HTb[hc][tt]]) for hc in range(DC)])
                        add_to_x(dm, tt, py)
                k.barrier()

        ONES_B = Tile(sb("ONESB", [128, 128], BF16))
        k.op(DVE, lambda: nc.vector.memset(ONES_B.t[:], 1.0), [], [ONES_B.buf])
        IDB = Tile(sb("IDB", [128, 128], BF16))
        k.cp(DVE, IDB.t[:], CST.t[:, 0:128], [CST.buf], [IDB.buf])
        k.barrier()

        for b in range(n_seq):
            load_x(b)
            for l in layer_ids:
                if l % 2 == 0:
                    if not SKIP_MIX:
                        conv_layer(l)
                    if not SKIP_FFN:
                        dense_ffn_layer(l)
                else:
                    if not SKIP_MIX:
                        mla_layer(l, b)
                    if not SKIP_FFN:
                        if SPARSE:
                            moe_layer_sparse(l)
                        else:
                            moe_layer(l)
            store_x(b)
        k.barrier()
    return nc


def prep_consts(inputs):
    g = np.zeros((128, NG), np.float32)
    nm = np.asarray(inputs["norm_mix"], np.float32)
    nf = np.asarray(inputs["norm_ffn"], np.float32)
    for l in range(DEPTH):
        g[:, GC_NMIX + l * DC: GC_NMIX + (l + 1) * DC] = nm[l].reshape(DC, 128).T
        g[:, GC_NFFN + l * DC: GC_NFFN + (l + 1) * DC] = nf[l].reshape(DC, 128).T
    cw = np.asarray(inputs["conv_w"], np.float32)
    for j in range(2):
        for kk in range(3):
            base = GC_CONV + (j * 3 + kk) * DC
            g[:, base:base + DC] = cw[j, kk].reshape(DC, 128).T
    qa = np.asarray(inputs["mla_q_a_norm"], np.float32)
    kva = np.asarray(inputs["mla_kv_a_norm"], np.float32)
    qn = np.asarray(inputs["mla_q_norm"], np.float32)
    kn = np.asarray(inputs["mla_k_norm"], np.float32)
    for j in range(2):
        g[:, GC_QA + j * 3: GC_QA + j * 3 + 3] = qa[j].reshape(3, 128).T
        g[:, GC_KVA + j * 2: GC_KVA + j * 2 + 2] = kva[j].reshape(2, 128).T
        g[:, GC_QN + j * 2] = qn[j, 0:128]
        g[0:64, GC_QN + j * 2 + 1] = qn[j, 128:192]
        g[:, GC_KN + j * 2] = kn[j, 0:128]
        g[64:128, GC_KN + j * 2 + 1] = kn[j, 128:192]
    inv_freq = (np.float32(10000.0) ** (-np.arange(0, ROPE, 2, dtype=np.float32) / np.float32(ROPE))).astype(np.float32)
    g[0:32, GC_FREQ] = inv_freq
    g[32:64, GC_FREQ] = inv_freq
    g[0:32, GC_SIGN] = 1.0
    g[32:64, GC_SIGN] = -1.0
    cst = np.zeros((128, 640), np.float32)
    cst[:, 512:640] = np.tile(np.arange(NE, dtype=np.float32) * CAPM, 16)[None, :]
    cst[:, 256:384] = (kk_ := np.arange(128)[:, None] < np.arange(128)[None, :]).astype(np.float32)
    cst[:, 384:512] = np.tile(np.arange(NE, dtype=np.float32) * CAPR, 16)[None, :]
    cst[:, 0:128] = np.eye(128, dtype=np.float32)
    kk_, qq_ = np.meshgrid(np.arange(128), np.arange(128), indexing="ij")
    cst[:, 128:256] = (qq_ >= kk_).astype(np.float32)
    return g, cst


W_NAMES = ["conv_w_in", "conv_w_out", "mla_w_down", "mla_w_uq", "mla_w_ukv", "mla_w_o",
           "ffn_w_gate", "ffn_w_up", "ffn_w_down", "moe_router", "moe_w_gate", "moe_w_up", "moe_w_down"]


def kernel(**inputs):
    x = np.ascontiguousarray(np.asarray(inputs["x"], np.float32))
    pos = np.ascontiguousarray(np.asarray(inputs["positions"], np.int32))
    g, cst = prep_consts(inputs)
    shared = {n: np.ascontiguousarray(np.asarray(inputs[n], np.float32)) for n in W_NAMES}
    shared["gcols"] = g
    shared["cst"] = cst
    nc = build_program(list(range(DEPTH)), SEQ_PER_CORE)
    in_maps = []
    for c in range(N_CORES):
        m = dict(shared)
        m["x"] = x[c * SEQ_PER_CORE:(c + 1) * SEQ_PER_CORE]
        m["positions"] = pos[c * SEQ_PER_CORE:(c + 1) * SEQ_PER_CORE]
        in_maps.append(m)
    res = run_bass_kernel_spmd(nc, in_maps, core_ids=list(range(N_CORES)))
    return np.concatenate([np.asarray(r["out"], np.float32) for r in res.results], axis=0)
```

```python
import contextlib
import numpy as np
import concourse.bass as bass
import concourse.mybir as mybir
from concourse.bass_utils import run_bass_kernel_spmd

F32 = mybir.dt.float32
BF16 = mybir.dt.bfloat16
I32 = mybir.dt.int32
AF = mybir.ActivationFunctionType
ALU = mybir.AluOpType

D = 1024
S = 2048
DEPTH = 4
NTT = S // 512
DC = D // 128
TCH = S // 128
NH = 8
DFF = 2816
NE = 8
DFE = 2048
QL = 384
KVL = 256
ROPE = 64
EPS = 1e-6
N_CORES = 8
SEQ_PER_CORE = 2
CAPR = 2048
CAPM = 640
SPARSE = True
OVT = CAPM
SKIP_MIX = False
SKIP_FFN = False

GC_NMIX = 0
GC_NFFN = GC_NMIX + DEPTH * DC
GC_CONV = GC_NFFN + DEPTH * DC
GC_QA = GC_CONV + 2 * 3 * DC
GC_KVA = GC_QA + 2 * 3
GC_QN = GC_KVA + 2 * 2
GC_KN = GC_QN + 4
GC_FREQ = GC_KN + 4
GC_SIGN = GC_FREQ + 1
NG = GC_SIGN + 1


class Sem:
    __slots__ = ("h", "val")

    def __init__(self, h):
        self.h = h
        self.val = 0


class Buf:
    __slots__ = ("w", "r")

    def __init__(self):
        self.w = None
        self.r = {}


class Eng:
    def __init__(self, name, e, sem):
        self.name = name
        self.e = e
        self.sem = sem
        self.waited = {}
        self.is_pe = name == "pe"
        self.ring = []
        self.ri = 0


class Tile:
    __slots__ = ("t", "buf")

    def __init__(self, t):
        self.t = t
        self.buf = Buf()


class RR:
    def __init__(self, items):
        self.items = items
        self.i = 0

    def get(self):
        it = self.items[self.i % len(self.items)]
        self.i += 1
        return it


class KB:
    def __init__(self, nc, es):
        self.nc = nc
        self.es = es

        def sem(n):
            return Sem(es.enter_context(nc.semaphore(n)))

        self.PE = Eng("pe", nc.tensor, sem("s_pe"))
        self.DVE = Eng("dve", nc.vector, sem("s_dve"))
        self.ACT = Eng("act", nc.scalar, sem("s_act"))
        self.POOL = Eng("pool", nc.gpsimd, sem("s_pool"))
        self.SP = Eng("sp", nc.sync, sem("s_sp"))
        self.POOL.ring = [sem(f"s_dp{i}") for i in range(12)]
        self.SP.ring = [sem(f"s_ds{i}") for i in range(12)]
        self.engines = [self.PE, self.DVE, self.ACT, self.POOL, self.SP]
        self.n_ins = 0
        self.n_guard = 0
        self.swq = []

    def wait(self, E, tag):
        s, v = tag
        if v <= 0 or E.waited.get(s, 0) >= v:
            return
        E.e.wait_ge(s.h, v)
        E.waited[s] = v

    def deps(self, E, reads, writes):
        for b in reads:
            if b.w is not None:
                if not (b.w[0] is E.sem and E.is_pe):
                    self.wait(E, b.w)
        for b in writes:
            if b.w is not None:
                if not (b.w[0] is E.sem and E.is_pe):
                    self.wait(E, b.w)
            for s, v in b.r.items():
                if s is E.sem and E.name != "pool":
                    continue
                self.wait(E, (s, v))

    def commit(self, tag, reads, writes):
        s, v = tag
        for b in reads:
            if b.r.get(s, 0) < v:
                b.r[s] = v
        for b in writes:
            b.w = tag
            b.r = {}

    def op(self, E, fn, reads=(), writes=()):
        self.deps(E, reads, writes)
        ins = fn()
        E.sem.val += 1
        ins.then_inc(E.sem.h, 1)
        self.commit((E.sem, E.sem.val), reads, writes)
        self.n_ins += 1

    def dma(self, Q, out_ap, in_ap, reads=(), writes=()):
        s = Q.ring[Q.ri % len(Q.ring)]
        Q.ri += 1
        self.wait(Q, (s, s.val))
        if Q is self.POOL:
            shp = list(out_ap.shape)
            nd = 1
            for d_ in shp[:-1]:
                nd *= int(d_)
            nd = max(1, nd // 16) * 2
            while self.swq and sum(n for _, n in self.swq) + nd > 512:
                tag, _ = self.swq.pop(0)
                self.wait(Q, tag)
        self.deps(Q, reads, writes)
        ins = Q.e.dma_start(out=out_ap, in_=in_ap)
        s.val += 16
        ins.then_inc(s.h, 16)
        self.commit((s, s.val), reads, writes)
        if Q is self.POOL:
            self.swq.append(((s, s.val), nd))
        self.n_ins += 1

    def idma(self, out_ap, out_off, in_ap, in_off, reads=(), writes=()):
        Q = self.POOL
        s = Q.ring[Q.ri % len(Q.ring)]
        Q.ri += 1
        self.wait(Q, (s, s.val))
        self.deps(Q, reads, writes)
        ins = Q.e.indirect_dma_start(out=out_ap, out_offset=out_off, in_=in_ap, in_offset=in_off)
        s.val += 16
        ins.then_inc(s.h, 16)
        self.commit((s, s.val), reads, writes)
        self.n_ins += 1

    def mm(self, out_ap, out_buf, parts):
        E = self.PE
        reads = [b for p in parts for b in p[2]]
        self.deps(E, reads, [out_buf])
        n = len(parts)
        ins = None
        for i, (l, r, _) in enumerate(parts):
            ins = E.e.matmul(out_ap, lhsT=l, rhs=r, start=(i == 0), stop=(i == n - 1))
            self.n_ins += 1
        E.sem.val += 1
        ins.then_inc(E.sem.h, 1)
        self.commit((E.sem, E.sem.val), reads, [out_buf])

    def mm1(self, out_ap, out_buf, l, r, reads, start, stop):
        E = self.PE
        self.deps(E, reads, [out_buf])
        ins = E.e.matmul(out_ap, lhsT=l, rhs=r, start=start, stop=stop)
        E.sem.val += 1
        ins.then_inc(E.sem.h, 1)
        self.commit((E.sem, E.sem.val), reads, [out_buf])
        self.n_ins += 1

    def transpose(self, out_ap, out_buf, in_ap, ident_ap, reads):
        E = self.PE
        self.deps(E, reads, [out_buf])
        ins = E.e.transpose(out_ap, in_ap, ident_ap)
        E.sem.val += 1
        ins.then_inc(E.sem.h, 1)
        self.commit((E.sem, E.sem.val), reads, [out_buf])
        self.n_ins += 1

    def all_sems(self):
        out = []
        for E in self.engines:
            out.append((E, E.sem))
            for r in E.ring:
                out.append((E, r))
        return out

    def guarded(self, regs, thr, body):
        nc = self.nc
        self.barrier()
        before = {id(s): s.val for _, s in self.all_sems()}
        caches = {E.name: dict(E.waited) for E in self.engines}
        saved = (self.POOL.ring, self.POOL.ri, self.SP.ring, self.SP.ri)
        self.swq = []
        self.POOL.ring = [Sem(self.es.enter_context(nc.semaphore(f"s_g{self.n_guard}_{i}"))) for i in range(4)]
        self.POOL.ri = 0
        self.SP.ring = []
        self.n_guard += 1
        with nc.If(nc.snap(regs) > thr):
            body()
        self.POOL.ring, self.POOL.ri, self.SP.ring, self.SP.ri = saved
        self.swq = []
        after = {id(s): s.val for _, s in self.all_sems()}
        with nc.Else():
            for E in self.engines:
                d = after[id(E.sem)] - before[id(E.sem)]
                while d > 0:
                    c = min(d, 200)
                    E.e.sem_inc(E.sem.h, c)
                    d -= c
        for E in self.engines:
            E.waited = caches[E.name]
        self.barrier()

    def barrier(self):
        tags = []
        for E in self.engines:
            tags.append((E.sem, E.sem.val))
            for s in E.ring:
                tags.append((s, s.val))
        for E in self.engines:
            for t in tags:
                if t[0] is E.sem:
                    continue
                self.wait(E, t)

    def act(self, out, in_, func, reads, writes, **kw):
        self.op(self.ACT, lambda: self.nc.scalar.activation(out=out, in_=in_, func=func, **kw), reads, writes)

    def stt(self, out, in0, scalar, in1, op0, op1, reads, writes):
        self.op(self.DVE, lambda: self.nc.vector.scalar_tensor_tensor(
            out=out, in0=in0, scalar=scalar, in1=in1, op0=op0, op1=op1), reads, writes)

    def tt(self, E, out, in0, in1, op, reads, writes):
        self.op(E, lambda: E.e.tensor_tensor(out=out, in0=in0, in1=in1, op=op), reads, writes)

    def ts(self, E, out, in0, s1, s2, op0, op1, reads, writes):
        if s2 is None:
            self.op(E, lambda: E.e.tensor_scalar(out=out, in0=in0, scalar1=s1, scalar2=None, op0=op0), reads, writes)
        else:
            self.op(E, lambda: E.e.tensor_scalar(out=out, in0=in0, scalar1=s1, scalar2=s2, op0=op0, op1=op1),
                    reads, writes)

    def cp(self, E, out, in_, reads, writes):
        if E is self.ACT:
            self.op(E, lambda: self.nc.scalar.copy(out=out, in_=in_), reads, writes)
        else:
            self.op(E, lambda: E.e.tensor_copy(out=out, in_=in_), reads, writes)


def build_program(layer_ids, n_seq):
    nc = bass.Bass("TRN2", target_bir_lowering=False)

    def din(name, shape, dt=F32):
        return nc.dram_tensor(name, list(shape), dt, kind="ExternalInput").ap()

    x_d = din("x", [n_seq, S, D])
    pos_d = din("positions", [n_seq, S], I32)
    gcols_d = din("gcols", [128, NG])
    cst_d = din("cst", [128, 640])
    w_conv_in = din("conv_w_in", [2, D, 3 * D])
    w_conv_out = din("conv_w_out", [2, D, D])
    w_mdown = din("mla_w_down", [2, D, QL + KVL + ROPE])
    w_muq = din("mla_w_uq", [2, QL, NH * 192])
    w_mukv = din("mla_w_ukv", [2, KVL, NH * 256])
    w_mo = din("mla_w_o", [2, D, D])
    w_fg = din("ffn_w_gate", [2, D, DFF])
    w_fu = din("ffn_w_up", [2, D, DFF])
    w_fd = din("ffn_w_down", [2, DFF, D])
    w_rt = din("moe_router", [2, D, NE])
    w_eg = din("moe_w_gate", [2, NE, D, DFE])
    w_eu = din("moe_w_up", [2, NE, D, DFE])
    w_ed = din("moe_w_down", [2, NE, DFE, D])
    out_d = nc.dram_tensor("out", [n_seq, S, D], F32, kind="ExternalOutput").ap()

    HS_d = nc.dram_tensor("hs_scr", [NE * CAPR, D], BF16, kind="Internal").ap()
    YS_d = nc.dram_tensor("ys_scr", [NE * CAPM, D], F32, kind="Internal").ap()
    CNT_d = nc.dram_tensor("cnt_scr", [1, 1], I32, kind="Internal").ap()

    es = contextlib.ExitStack()
    with es:
        k = KB(nc, es)
        PE, DVE, ACT, POOL, SP = k.PE, k.DVE, k.ACT, k.POOL, k.SP

        uid = [0]

        def sb(name, shape, dt, stack=es):
            uid[0] += 1
            return stack.enter_context(nc.sbuf_tensor(f"{name}_{uid[0]}", list(shape), dt))

        XT = sb("XT", [128, DC, S], F32)
        XTb = [[Buf() for _ in range(NTT)] for _ in range(DC)]
        HT = sb("HT", [128, DC, S], BF16)
        HTb = [[Buf() for _ in range(NTT)] for _ in range(DC)]
        GC = Tile(sb("GC", [128, NG], F32))
        CST = Tile(sb("CST", [128, 640], F32))
        ONES = Tile(sb("ONES", [128, 128], F32))
        EPSC = Tile(sb("EPSC", [128, 1], F32))
        TRI = Tile(sb("TRI", [128, 128], BF16))
        psum = [Tile(es.enter_context(nc.psum_tensor(f"ps{i}", [128, 512], F32))) for i in range(8)]

        ident = CST.t[:, 0:128]

        def gcol(i, p0=0, p1=128):
            return GC.t[p0:p1, i:i + 1]

        k.dma(SP, GC.t[:], gcols_d, [], [GC.buf])
        k.dma(SP, CST.t[:], cst_d, [], [CST.buf])
        k.op(DVE, lambda: nc.vector.memset(ONES.t[:], 1.0), [], [ONES.buf])
        k.op(DVE, lambda: nc.vector.memset(EPSC.t[:], EPS), [], [EPSC.buf])
        k.cp(DVE, TRI.t[:], CST.t[:, 128:256], [CST.buf], [TRI.buf])
        sc = float(192.0 ** -0.5)
        k.ts(DVE, GC.t[:, GC_QN:GC_QN + 4], GC.t[:, GC_QN:GC_QN + 4], sc, None, ALU.mult, None, [GC.buf], [GC.buf])

        def rms_stats(stack_pools, srcs, nfeat):
            SQ, SSP, RT, RS = stack_pools
            ss = SSP.get()
            n = len(srcs)
            for i, (ap, b, p0, p1) in enumerate(srcs):
                sq = SQ.get()
                k.act(sq.t[p0:p1, :], ap, AF.Square, [b], [sq.buf])
                k.mm1(ss.t[:], ss.buf, ONES_B.t[p0:p1, :], sq.t[p0:p1, :], [ONES_B.buf, sq.buf], i == 0, i == n - 1)
            rt = RT.get()
            k.act(rt.t[:], ss.t[:], AF.Ln, [ss.buf, EPSC.buf], [rt.buf], scale=1.0 / nfeat, bias=EPSC.t[:])
            rs = RS.get()
            k.act(rs.t[:], rt.t[:], AF.Exp, [rt.buf], [rs.buf], scale=-0.5)
            return rs

        def main_norm(l, which, pools, hf_cb=None):
            gbase = (GC_NMIX if which == 0 else GC_NFFN) + l * DC

            def stats(tt):
                cols = slice(tt * 512, (tt + 1) * 512)
                return rms_stats(pools, [(XT[:, c, cols], XTb[c][tt], 0, 128) for c in range(DC)], D)

            rs_next = stats(0)
            for tt in range(NTT):
                cols = slice(tt * 512, (tt + 1) * 512)
                rs = rs_next
                if tt + 1 < NTT:
                    rs_next = stats(tt + 1)
                if hf_cb is None:
                    for c in range(DC):
                        k.stt(HT[:, c, cols], XT[:, c, cols], gcol(gbase + c), rs.t[:], ALU.mult, ALU.mult,
                              [XTb[c][tt], GC.buf, rs.buf], [HTb[c][tt]])
                else:
                    hf_cb(tt, cols, rs, gbase)

        def add_to_x(c, tt, ps):
            cols = slice(tt * 512, (tt + 1) * 512)
            k.tt(DVE, XT[:, c, cols], XT[:, c, cols], ps.t[:], ALU.add, [XTb[c][tt], ps.buf], [XTb[c][tt]])

        def tmp_pool(stack, name, n, dt=F32, shape=(128, 512)):
            return RR([Tile(sb(f"{name}{i}", list(shape), dt, stack)) for i in range(n)])

        def load_x(b):
            with contextlib.ExitStack() as st:
                XIN = tmp_pool(st, "xin", 4, F32, (128, D))
                PSP = RR(psum)
                for tc in range(TCH):
                    xin = XIN.get()
                    k.dma(SP, xin.t[:], x_d[b, tc * 128:(tc + 1) * 128, :], [], [xin.buf])
                    tt = tc // 4
                    for c in range(DC):
                        ps = PSP.get()
                        k.transpose(ps.t[:, 0:128], ps.buf, xin.t[:, c * 128:(c + 1) * 128], ident, [xin.buf, CST.buf])
                        E = DVE if c % 2 == 0 else ACT
                        k.cp(E, XT[:, c, tc * 128:(tc + 1) * 128], ps.t[:, 0:128], [ps.buf], [XTb[c][tt]])
                k.barrier()

        def store_x(b):
            with contextlib.ExitStack() as st:
                XO = tmp_pool(st, "xo", 4, F32, (128, D))
                PSP = RR(psum)
                for tc in range(TCH):
                    xo = XO.get()
                    tt = tc // 4
                    for c in range(DC):
                        ps = PSP.get()
                        k.transpose(ps.t[:, 0:128], ps.buf, XT[:, c, tc * 128:(tc + 1) * 128], ident,
                                    [XTb[c][tt], CST.buf])
                        E = DVE if c % 2 == 0 else ACT
                        k.cp(E, xo.t[:, c * 128:(c + 1) * 128], ps.t[:, 0:128], [ps.buf], [xo.buf])
                    k.dma(SP, out_d[b, tc * 128:(tc + 1) * 128, :], xo.t[:], [xo.buf], [])
                k.barrier()

        def conv_layer(l):
            j = l // 2
            with contextlib.ExitStack() as st:
                WIN = Tile(sb("c_win", [128, DC, 3 * D], BF16, st))
                WOUT = Tile(sb("c_wout", [128, DC, D], BF16, st))
                VB = Tile(sb("c_vb", [128, DC, 514], F32, st))
                GT = tmp_pool(st, "c_gt", 1, BF16, (128, DC, 512))
                SQ = tmp_pool(st, "c_sq", 2, BF16)
                RT = tmp_pool(st, "c_rt", 1)
                RS = tmp_pool(st, "c_rs", 2)
                CC = tmp_pool(st, "c_cc", 2)
                T0 = tmp_pool(st, "c_t0", 2)
                win_v = w_conv_in[j].rearrange("(c p) f -> p c f", p=128)
                WINb = [[Buf() for _ in range(2)] for _ in range(3)]
                for hf_ in range(2):
                    for g in range(3):
                        c0 = g * D + hf_ * 512
                        k.dma(POOL, WIN.t[:, :, c0:c0 + 512], win_v[:, :, c0:c0 + 512], [], [WINb[g][hf_]])
                wout_v = w_conv_out[j].rearrange("(c p) f -> p c f", p=128)
                for c in range(DC):
                    k.dma(POOL, WOUT.t[:, c, :], wout_v[:, c, :], [], [WOUT.buf])
                k.op(DVE, lambda: nc.vector.memset(VB.t[:, :, 0:2], 0.0), [], [VB.buf])
                main_norm(l, 0, (SQ, RR(psum[6:8]), RT, RS))
                PSB = RR(psum[0:6])
                PSY = RR(psum[6:8])
                cw = GC_CONV + j * 3 * DC
                for tt in range(NTT):
                    cols = slice(tt * 512, (tt + 1) * 512)
                    gt = GT.get()
                    for fc in range(DC):
                        pb, pc, pu = PSB.get(), PSB.get(), PSB.get()
                        for g, ps in enumerate((pb, pc, pu)):
                            k.mm(ps.t[:], ps.buf,
                                 [(WIN.t[:, kc, g * D + fc * 128: g * D + (fc + 1) * 128], HT[:, kc, cols],
                                   [WINb[g][fc // 4], HTb[kc][tt]]) for kc in range(DC)])
                        cc = CC.get()
                        k.cp(ACT, cc.t[:], pc.t[:], [pc.buf], [cc.buf])
                        k.tt(DVE, VB.t[:, fc, 2:514], cc.t[:], pu.t[:], ALU.mult, [cc.buf, pu.buf], [VB.buf])
                        t0 = T0.get()
                        k.act(t0.t[:], VB.t[:, fc, 2:514], AF.Copy, [VB.buf, GC.buf], [t0.buf],
                              scale=gcol(cw + 2 * DC + fc))
                        k.stt(t0.t[:], VB.t[:, fc, 1:513], gcol(cw + 1 * DC + fc), t0.t[:], ALU.mult, ALU.add,
                              [VB.buf, GC.buf, t0.buf], [t0.buf])
                        k.stt(t0.t[:], VB.t[:, fc, 0:512], gcol(cw + 0 * DC + fc), t0.t[:], ALU.mult, ALU.add,
                              [VB.buf, GC.buf, t0.buf], [t0.buf])
                        k.tt(DVE, gt.t[:, fc, :], t0.t[:], pb.t[:], ALU.mult, [t0.buf, pb.buf], [gt.buf])
                    k.cp(DVE, VB.t[:, :, 0:2], VB.t[:, :, 512:514], [VB.buf], [VB.buf])
                    for dm in range(DC):
                        py = PSY.get()
                        k.mm(py.t[:], py.buf,
                             [(WOUT.t[:, fc, dm * 128:(dm + 1) * 128], gt.t[:, fc, :], [WOUT.buf, gt.buf])
                              for fc in range(DC)])
                        add_to_x(dm, tt, py)
                k.barrier()

        def ffn_blocks(st, blocks, gate_mul=None, slots=None):
            if slots is None:
                GUS = [Tile(sb(f"f_gu{i}", [128, 8192], BF16, st)) for i in range(2)]
                WDS = [Tile(sb(f"f_wd{i}", [128, 4096], BF16, st)) for i in range(2)]
            else:
                GUS, WDS = slots
            AT = tmp_pool(st, "f_at", 2, BF16, (128, 4, 512))
            SS = tmp_pool(st, "f_ss", 2)
            TT = tmp_pool(st, "f_tt", 2)
            PSG = RR(psum[0:4])
            PSY = RR(psum[4:8])
            nb = len(blocks)
            units = [(i, tt) for i in range(nb) for tt in range(NTT)]
            gu_l, wd_l = {}, {}

            def load_gu(i):
                if i >= nb or i in gu_l:
                    return
                blk = blocks[i]
                slot = GUS[i % 2]
                w = blk["nfc"] * 128
                wg = slot.t[:, 0:DC * w].rearrange("p (c f) -> p c f", c=DC)
                wu = slot.t[:, 4096:4096 + DC * w].rearrange("p (c f) -> p c f", c=DC)
                k.dma(POOL, wg, blk["wg"], [], [slot.buf])
                k.dma(POOL, wu, blk["wu"], [], [slot.buf])
                gu_l[i] = (slot, wg, wu)

            def load_wd(i):
                if i >= nb or i in wd_l:
                    return
                blk = blocks[i]
                slot = WDS[i % 2]
                nfc = blk["nfc"]
                wd = slot.t[:, 0:nfc * D].rearrange("p (c f) -> p c f", c=nfc)
                k.dma(POOL, wd, blk["wd"], [], [slot.buf])
                wd_l[i] = (slot, wd)

            load_gu(0)
            load_gu(1)
            load_wd(0)
            load_wd(1)

            def stage1(u):
                i, tt = units[u]
                blk = blocks[i]
                slot, wg, wu = gu_l[i]
                nfc = blk["nfc"]
                G = gate_mul(blk) if gate_mul is not None else None
                cols = slice(tt * 512, (tt + 1) * 512)
                at = AT.get()
                for fc in range(nfc):
                    pg, pu = PSG.get(), PSG.get()
                    k.mm(pg.t[:], pg.buf, [(wg[:, kc, fc * 128:(fc + 1) * 128], HT[:, kc, cols],
                                            [slot.buf, HTb[kc][tt]]) for kc in range(DC)])
                    k.mm(pu.t[:], pu.buf, [(wu[:, kc, fc * 128:(fc + 1) * 128], HT[:, kc, cols],
                                            [slot.buf, HTb[kc][tt]]) for kc in range(DC)])
                    s_ = SS.get()
                    k.act(s_.t[:], pg.t[:], AF.Silu, [pg.buf], [s_.buf])
                    if G is None:
                        k.tt(DVE, at.t[:, fc, :], s_.t[:], pu.t[:], ALU.mult, [s_.buf, pu.buf], [at.buf])
                    else:
                        t = TT.get()
                        k.tt(DVE, t.t[:], G.t[:, cols], pu.t[:], ALU.mult, [G.buf, pu.buf], [t.buf])
                        k.tt(POOL, at.t[:, fc, :], s_.t[:], t.t[:], ALU.mult, [s_.buf, t.buf], [at.buf])
                return at

            def stage2(u, at):
                i, tt = units[u]
                blk = blocks[i]
                slot, wd = wd_l[i]
                for dm in range(DC):
                    py = PSY.get()
                    k.mm(py.t[:], py.buf, [(wd[:, fc, dm * 128:(dm + 1) * 128], at.t[:, fc, :],
                                            [slot.buf, at.buf]) for fc in range(blk["nfc"])])
                    add_to_x(dm, tt, py)

            at_next = stage1(0)
            for u in range(len(units)):
                at = at_next
                if u + 1 < len(units):
                    at_next = stage1(u + 1)
                    if units[u + 1][1] == NTT - 1:
                        load_gu(units[u + 1][0] + 2)
                stage2(u, at)
                i, tt = units[u]
                if tt == NTT - 1:
                    load_wd(i + 2)

        zeroed = [False]

        def dense_ffn_layer(l):
            j = l // 2
            with contextlib.ExitStack() as st:
                if not zeroed[0]:
                    zeroed[0] = True
                    ZB = Tile(sb("zb", [128, 5, D], BF16, st))
                    k.op(POOL, lambda: nc.gpsimd.memset(ZB.t[:], 0.0), [], [ZB.buf])
                    for e in range(NE):
                        k.dma(SP, HS_d[e * CAPR:e * CAPR + CAPM, :].rearrange("(c p) f -> p c f", p=128), ZB.t[:],
                              [ZB.buf], [HSb[0][0]])
                SQ = tmp_pool(st, "d_sq", 2, BF16)
                RT = tmp_pool(st, "d_rt", 1)
                RS = tmp_pool(st, "d_rs", 2)
                main_norm(l, 1, (SQ, RR(psum[6:8]), RT, RS))
                gv = w_fg[j].rearrange("(c p) f -> p c f", p=128)
                uv = w_fu[j].rearrange("(c p) f -> p c f", p=128)
                blocks = []
                f0 = 0
                while f0 < DFF:
                    w = min(512, DFF - f0)
                    blocks.append(dict(
                        wg=gv[:, :, f0:f0 + w], wu=uv[:, :, f0:f0 + w],
                        wd=w_fd[j, f0:f0 + w, :].rearrange("(c p) d -> p c d", p=128), nfc=w // 128))
                    f0 += w
                ffn_blocks(st, blocks)
                k.barrier()

        def moe_layer(l):
            j = l // 2
            with contextlib.ExitStack() as st:
                SQ = tmp_pool(st, "m_sq", 2, BF16)
                RT = tmp_pool(st, "m_rt", 1)
                RS = tmp_pool(st, "m_rs", 2)
                HF = tmp_pool(st, "m_hf", 3)
                RW = Tile(sb("m_rw", [128, DC, NE], F32, st))
                GATES = Tile(sb("m_gates", [128, TCH, NE], F32, st))
                GB = [Tile(sb(f"m_gb{i}", [128, S], F32, st)) for i in range(2)]
                GL = tmp_pool(st, "m_gl", 2, F32, (128, 128))
                SM = tmp_pool(st, "m_sm", 12, F32, (128, NE))
                SC = tmp_pool(st, "m_sc", 12, F32, (128, 1))
                k.dma(SP, RW.t[:], w_rt[j].rearrange("(c p) e -> p c e", p=128), [], [RW.buf])

                def hf_cb(tt, cols, rs, gbase):
                    lps = [psum[q] for q in range(4)]
                    for c in range(DC):
                        hf = HF.get()
                        k.stt(hf.t[:], XT[:, c, cols], gcol(gbase + c), rs.t[:], ALU.mult, ALU.mult,
                              [XTb[c][tt], GC.buf, rs.buf], [hf.buf])
                        k.cp(ACT, HT[:, c, cols], hf.t[:], [hf.buf], [HTb[c][tt]])
                        for q in range(4):
                            k.mm1(lps[q].t[:, 0:NE], lps[q].buf, hf.t[:, q * 128:(q + 1) * 128], RW.t[:, c, :],
                                  [hf.buf, RW.buf], c == 0, c == DC - 1)
                    for q in range(4):
                        tc = tt * 4 + q
                        lg = SM.get()
                        k.cp(DVE, lg.t[:], lps[q].t[:, 0:NE], [lps[q].buf], [lg.buf])
                        m1 = SC.get()
                        k.op(DVE, lambda: nc.vector.reduce_max(out=m1.t[:], in_=lg.t[:], axis=mybir.AxisListType.X),
                             [lg.buf], [m1.buf])
                        eq1 = SM.get()
                        k.ts(DVE, eq1.t[:], lg.t[:], m1.t[:], None, ALU.is_equal, None, [lg.buf, m1.buf], [eq1.buf])
                        l2 = SM.get()
                        k.stt(l2.t[:], eq1.t[:], -1e30, lg.t[:], ALU.mult, ALU.add, [eq1.buf, lg.buf], [l2.buf])
                        m2 = SC.get()
                        k.op(DVE, lambda: nc.vector.reduce_max(out=m2.t[:], in_=l2.t[:], axis=mybir.AxisListType.X),
                             [l2.buf], [m2.buf])
                        eq2 = SM.get()
                        k.ts(DVE, eq2.t[:], l2.t[:], m2.t[:], None, ALU.is_equal, None, [l2.buf, m2.buf], [eq2.buf])
                        dd = SC.get()
                        k.tt(DVE, dd.t[:], m2.t[:], m1.t[:], ALU.subtract, [m2.buf, m1.buf], [dd.buf])
                        e2 = SC.get()
                        k.act(e2.t[:], dd.t[:], AF.Exp, [dd.buf], [e2.buf])
                        den = SC.get()
                        k.ts(DVE, den.t[:], e2.t[:], 1.0, None, ALU.add, None, [e2.buf], [den.buf])
                        g1 = SC.get()
                        k.op(DVE, lambda: nc.vector.reciprocal(out=g1.t[:], in_=den.t[:]), [den.buf], [g1.buf])
                        g2 = SC.get()
                        k.tt(DVE, g2.t[:], e2.t[:], g1.t[:], ALU.mult, [e2.buf, g1.buf], [g2.buf])
                        ga = SM.get()
                        k.ts(DVE, ga.t[:], eq1.t[:], g1.t[:], None, ALU.mult, None, [eq1.buf, g1.buf], [ga.buf])
                        k.stt(GATES.t[:, tc, :], eq2.t[:], g2.t[:], ga.t[:], ALU.mult, ALU.add,
                              [eq2.buf, g2.buf, ga.buf], [GATES.buf])

                main_norm(l, 1, (SQ, RR(psum[6:8]), RT, RS), hf_cb)

                PSG2 = RR(psum[4:8])
                gstate = {"i": 0}

                def gate_mul(blk):
                    if blk["first"] and gstate.get("done") != blk["e"]:
                        gstate["done"] = blk["e"]
                        e = blk["e"]
                        G = GB[gstate["i"] % 2]
                        gstate["i"] += 1
                        for tt in range(NTT):
                            ps = PSG2.get()
                            for q in range(4):
                                tc = tt * 4 + q
                                gl = GL.get()
                                k.ts(DVE, gl.t[:], ONES.t[:], GATES.t[:, tc, e:e + 1], None, ALU.mult, None,
                                     [ONES.buf, GATES.buf], [gl.buf])
                                k.mm1(ps.t[:, q * 128:(q + 1) * 128], ps.buf, gl.t[:], ident, [gl.buf, CST.buf],
                                      True, True)
                            k.cp(ACT, G.t[:, tt * 512:(tt + 1) * 512], ps.t[:], [ps.buf], [G.buf])
                        gstate["G"] = G
                    return gstate["G"]

                blocks = []
                for e in range(NE):
                    gv = w_eg[j, e].rearrange("(c p) f -> p c f", p=128)
                    uv = w_eu[j, e].rearrange("(c p) f -> p c f", p=128)
                    for f0 in range(0, DFE, 512):
                        blocks.append(dict(
                            wg=gv[:, :, f0:f0 + 512], wu=uv[:, :, f0:f0 + 512],
                            wd=w_ed[j, e, f0:f0 + 512, :].rearrange("(c p) d -> p c d", p=128), nfc=4,
                            e=e, first=(f0 == 0)))
                ffn_blocks(st, blocks, gate_mul)
                k.barrier()


        HSb = [[Buf() for _ in range(2)] for _ in range(TCH)]
        CNTb = Buf()
        YSb = [Buf() for _ in range(NE)]
        HTM = HT[:].rearrange("p c s -> p (c s)").rearrange("p (t d) -> p t d", t=TCH)
        HTMb = [Buf() for _ in range(TCH)]
        LT = CST.t[:, 256:384]
        EBASE = CST.t[:, 384:512]
        EBASE2 = CST.t[:, 512:640]

        def moe_layer_sparse(l):
            j = l // 2
            with contextlib.ExitStack() as st:
                RW = Tile(sb("s_rw", [128, DC, NE], F32, st))
                EQ1 = Tile(sb("s_eq1", [128, TCH, NE], F32, st))
                EQ2 = Tile(sb("s_eq2", [128, TCH, NE], F32, st))
                G1 = Tile(sb("s_g1", [128, TCH], F32, st))
                G2 = Tile(sb("s_g2", [128, TCH], F32, st))
                PI1 = Tile(sb("s_pi1", [128, TCH], I32, st))
                PI2 = Tile(sb("s_pi2", [128, TCH], I32, st))
                PJ1 = Tile(sb("s_pj1", [128, TCH], I32, st))
                PJ2 = Tile(sb("s_pj2", [128, TCH], I32, st))
                G1E = Tile(sb("s_g1e", [128, TCH], F32, st))
                G2E = Tile(sb("s_g2e", [128, TCH], F32, st))
                GO = Tile(sb("s_go", [128, TCH, NE], F32, st))
                k.dma(SP, RW.t[:], w_rt[j].rearrange("(c p) e -> p c e", p=128), [], [RW.buf])
                GUS = [Tile(sb(f"s_gu{i}", [128, 8192], BF16, st)) for i in range(2)]
                WDS = [Tile(sb(f"s_wd{i}", [128, 4096], BF16, st)) for i in range(2)]
                PI1b = [Buf() for _ in range(NTT)]
                PI2b = [Buf() for _ in range(NTT)]
                blocks = []
                for e in range(NE):
                    gv = w_eg[j, e].rearrange("(c p) f -> p c f", p=128)
                    uv = w_eu[j, e].rearrange("(c p) f -> p c f", p=128)
                    for bi, f0 in enumerate(range(0, DFE, 512)):
                        blocks.append(dict(
                            wg=gv[:, :, f0:f0 + 512], wu=uv[:, :, f0:f0 + 512],
                            wd=w_ed[j, e, f0:f0 + 512, :].rearrange("(c p) d -> p c d", p=128), e=e, bi=bi))
                nb = len(blocks)
                gu_l, wd_l = {}, {}

                def load_gu(i):
                    if i >= nb or i in gu_l:
                        return
                    blk = blocks[i]
                    slot = GUS[i % 2]
                    wg = slot.t[:, 0:4096].rearrange("p (c f) -> p c f", c=DC)
                    wu = slot.t[:, 4096:8192].rearrange("p (c f) -> p c f", c=DC)
                    k.dma(POOL, wg, blk["wg"], [], [slot.buf])
                    k.dma(POOL, wu, blk["wu"], [], [slot.buf])
                    gu_l[i] = (slot, wg, wu)

                def load_wd(i):
                    if i >= nb or i in wd_l:
                        return
                    blk = blocks[i]
                    slot = WDS[i % 2]
                    wd = slot.t[:, 0:4096].rearrange("p (c f) -> p c f", c=4)
                    k.dma(POOL, wd, blk["wd"], [], [slot.buf])
                    wd_l[i] = (slot, wd)

                load_gu(0)
                load_gu(1)
                load_wd(0)
                load_wd(1)

                with contextlib.ExitStack() as st2:
                    SQ = tmp_pool(st2, "s_sq", 2, BF16)
                    RT = tmp_pool(st2, "s_rt", 1)
                    RS = tmp_pool(st2, "s_rs", 2)
                    HF = tmp_pool(st2, "s_hf", 3)
                    SM = tmp_pool(st2, "s_sm", 8, F32, (128, NE))
                    SC = tmp_pool(st2, "s_sc", 12, F32, (128, 1))
                    MS = Tile(sb("s_ms", [128, TCH * NE], F32, st2))
                    TOT = Tile(sb("s_tot", [128, TCH, NE], F32, st2))
                    CUM = Tile(sb("s_cum", [128, TCH, NE], F32, st2))
                    PB = Tile(sb("s_pb", [128, TCH, NE], F32, st2))
                    PM = Tile(sb("s_pm", [128, TCH, NE], F32, st2))
                    PB2 = Tile(sb("s_pb2", [128, TCH, NE], F32, st2))
                    PF = Tile(sb("s_pf", [128, TCH], F32, st2))
                    OV = Tile(sb("s_ov", [128, TCH, NE], F32, st2))
                    KP = Tile(sb("s_kp", [128, TCH], F32, st2))
                    NEC = Tile(sb("s_nec", [128, NE], F32, st2))
                    MXF = Tile(sb("s_mxf", [128, 1], F32, st2))
                    MXI = Tile(sb("s_mxi", [128, 1], I32, st2))
                    PTR = RR(psum[4:6])
                    LG = tmp_pool(st2, "s_lg", 8, F32, (128, NE))
                    pending = []

                    def hf_cb(tt, cols, rs, gbase):
                        lps = [psum[q] for q in range(4)]
                        for c in range(DC):
                            hf = HF.get()
                            k.stt(hf.t[:], XT[:, c, cols], gcol(gbase + c), rs.t[:], ALU.mult, ALU.mult,
                                  [XTb[c][tt], GC.buf, rs.buf], [hf.buf])
                            for q in range(4):
                                k.mm1(lps[q].t[:, 0:NE], lps[q].buf, hf.t[:, q * 128:(q + 1) * 128], RW.t[:, c, :],
                                      [hf.buf, RW.buf], c == 0, c == DC - 1)
                            ptr = PTR.get()
                            for q in range(4):
                                k.transpose(ptr.t[:, q * 128:(q + 1) * 128], ptr.buf, hf.t[:, q * 128:(q + 1) * 128],
                                            ident, [hf.buf, CST.buf])
                            k.cp(ACT, HTM[:, tt * 4:(tt + 1) * 4, c * 128:(c + 1) * 128],
                                 ptr.t[:].rearrange("p (q d) -> p q d", q=4), [ptr.buf],
                                 [HTMb[tt * 4 + q] for q in range(4)])
                        lgs = []
                        for q in range(4):
                            lg = LG.get()
                            k.cp(DVE, lg.t[:], lps[q].t[:, 0:NE], [lps[q].buf], [lg.buf])
                            lgs.append(lg)
                        if pending:
                            pending.pop()()
                        pending.append(lambda: route(tt, lgs))

                    def route(tt, lgs):
                        for q in range(4):
                            tc = tt * 4 + q
                            lg = lgs[q]
                            m1 = SC.get()
                            k.op(DVE, lambda: nc.vector.reduce_max(out=m1.t[:], in_=lg.t[:], axis=mybir.AxisListType.X),
                                 [lg.buf], [m1.buf])
                            k.ts(DVE, EQ1.t[:, tc, :], lg.t[:], m1.t[:], None, ALU.is_equal, None,
                                 [lg.buf, m1.buf], [EQ1.buf])
                            l2 = SM.get()
                            k.stt(l2.t[:], EQ1.t[:, tc, :], -1e30, lg.t[:], ALU.mult, ALU.add, [EQ1.buf, lg.buf], [l2.buf])
                            m2 = SC.get()
                            k.op(DVE, lambda: nc.vector.reduce_max(out=m2.t[:], in_=l2.t[:], axis=mybir.AxisListType.X),
                                 [l2.buf], [m2.buf])
                            k.ts(DVE, EQ2.t[:, tc, :], l2.t[:], m2.t[:], None, ALU.is_equal, None,
                                 [l2.buf, m2.buf], [EQ2.buf])
                            dd = SC.get()
                            k.tt(DVE, dd.t[:], m2.t[:], m1.t[:], ALU.subtract, [m2.buf, m1.buf], [dd.buf])
                            e2 = SC.get()
                            k.act(e2.t[:], dd.t[:], AF.Exp, [dd.buf], [e2.buf])
                            den = SC.get()
                            k.ts(DVE, den.t[:], e2.t[:], 1.0, None, ALU.add, None, [e2.buf], [den.buf])
                            k.op(DVE, lambda: nc.vector.reciprocal(out=G1.t[:, tc:tc + 1], in_=den.t[:]),
                                 [den.buf], [G1.buf])
                            k.tt(DVE, G2.t[:, tc:tc + 1], e2.t[:], G1.t[:, tc:tc + 1], ALU.mult, [e2.buf, G1.buf], [G2.buf])
                        ranks(tt)

                    def ranks(tt):
                        fs = slice(32 * tt, 32 * tt + 32)
                        ts_ = slice(4 * tt, 4 * tt + 4)
                        eq1f = EQ1.t[:].rearrange("p t e -> p (t e)")
                        eq2f = EQ2.t[:].rearrange("p t e -> p (t e)")
                        k.tt(DVE, MS.t[:, fs], eq1f[:, fs], eq2f[:, fs], ALU.add, [EQ1.buf, EQ2.buf], [MS.buf])
                        pe_, pt_ = PTR.get(), PTR.get()
                        k.mm1(pe_.t[:, 0:32], pe_.buf, LT, MS.t[:, fs], [CST.buf, MS.buf], True, True)
                        k.mm1(pt_.t[:, 0:32], pt_.buf, ONES.t[:], MS.t[:, fs], [ONES.buf, MS.buf], True, True)
                        k.cp(DVE, TOT.t[:].rearrange("p t e -> p (t e)")[:, fs], pt_.t[:, 0:32], [pt_.buf], [TOT.buf])
                        if tt == 0:
                            k.op(DVE, lambda: nc.vector.memset(CUM.t[:, 0, :], 0.0), [], [CUM.buf])
                        for tc in range(4 * tt + 1, min(4 * tt + 5, TCH)):
                            k.tt(DVE, CUM.t[:, tc, :], CUM.t[:, tc - 1, :], TOT.t[:, tc - 1, :], ALU.add,
                                 [CUM.buf, TOT.buf], [CUM.buf])
                        pbf = PB.t[:].rearrange("p t e -> p (t e)")[:, fs]
                        k.tt(DVE, pbf, CUM.t[:].rearrange("p t e -> p (t e)")[:, fs], pe_.t[:, 0:32], ALU.add,
                             [CUM.buf, pe_.buf], [PB.buf])
                        ovf = OV.t[:].rearrange("p t e -> p (t e)")[:, fs]
                        k.ts(DVE, ovf, pbf, float(OVT) - 0.5, None, ALU.is_gt, None, [PB.buf], [OV.buf])
                        pb2 = PB2.t[:].rearrange("p t e -> p (t e)")[:, fs]
                        k.tt(DVE, pb2, pbf, EBASE2[:, fs], ALU.add, [PB.buf, CST.buf], [PB2.buf])
                        k.tt(DVE, pbf, pbf, EBASE[:, fs], ALU.add, [PB.buf, CST.buf], [PB.buf])
                        pmf = PM.t[:].rearrange("p t e -> p (t e)")[:, fs]
                        for EQ, PI, PIb, PJ, G, GE in ((EQ1, PI1, PI1b, PJ1, G1, G1E), (EQ2, PI2, PI2b, PJ2, G2, G2E)):
                            eqf = EQ.t[:].rearrange("p t e -> p (t e)")[:, fs]
                            k.tt(DVE, pmf, eqf, pbf, ALU.mult, [EQ.buf, PB.buf], [PM.buf])
                            k.op(DVE, lambda: nc.vector.reduce_sum(out=PF.t[:, ts_], in_=PM.t[:, ts_, :],
                                                                   axis=mybir.AxisListType.X), [PM.buf], [PF.buf])
                            k.cp(DVE, PI.t[:, ts_], PF.t[:, ts_], [PF.buf], [PIb[tt]])
                            k.tt(DVE, pmf, eqf, ovf, ALU.mult, [EQ.buf, OV.buf], [PM.buf])
                            k.op(DVE, lambda: nc.vector.reduce_sum(out=KP.t[:, ts_], in_=PM.t[:, ts_, :],
                                                                   axis=mybir.AxisListType.X), [PM.buf], [KP.buf])
                            k.ts(DVE, KP.t[:, ts_], KP.t[:, ts_], -1.0, 1.0, ALU.mult, ALU.add, [KP.buf], [KP.buf])
                            k.tt(DVE, pmf, eqf, pb2, ALU.mult, [EQ.buf, PB2.buf], [PM.buf])
                            k.op(DVE, lambda: nc.vector.reduce_sum(out=PF.t[:, ts_], in_=PM.t[:, ts_, :],
                                                                   axis=mybir.AxisListType.X), [PM.buf], [PF.buf])
                            k.tt(DVE, PF.t[:, ts_], PF.t[:, ts_], KP.t[:, ts_], ALU.mult, [PF.buf, KP.buf], [PF.buf])
                            k.cp(DVE, PJ.t[:, ts_], PF.t[:, ts_], [PF.buf], [PJ.buf])
                            k.tt(DVE, GE.t[:, ts_], G.t[:, ts_], KP.t[:, ts_], ALU.mult, [G.buf, KP.buf], [GE.buf])
                        for tc in range(4 * tt, 4 * tt + 4):
                            k.ts(DVE, GO.t[:, tc, :], EQ1.t[:, tc, :], G1.t[:, tc:tc + 1], None, ALU.mult, None,
                                 [EQ1.buf, G1.buf], [GO.buf])
                            k.stt(GO.t[:, tc, :], EQ2.t[:, tc, :], G2.t[:, tc:tc + 1], GO.t[:, tc, :], ALU.mult, ALU.add,
                                  [EQ2.buf, G2.buf, GO.buf], [GO.buf])
                        gof = GO.t[:].rearrange("p t e -> p (t e)")[:, fs]
                        k.tt(DVE, gof, gof, ovf, ALU.mult, [GO.buf, OV.buf], [GO.buf])
                        for tc in range(4 * tt, 4 * tt + 4):
                            for kk, (PI, PIb) in enumerate(((PI1, PI1b), (PI2, PI2b))):
                                k.idma(HS_d[:, :], bass.IndirectOffsetOnAxis(ap=PI.t[:, tc:tc + 1], axis=0),
                                       HTM[:, tc, :], None, [HTMb[tc], PIb[tt]], [HSb[tc][kk]])
                        if tt == NTT - 1:
                            k.tt(DVE, NEC.t[:], CUM.t[:, TCH - 1, :], TOT.t[:, TCH - 1, :], ALU.add,
                                 [CUM.buf, TOT.buf], [NEC.buf])
                            k.op(DVE, lambda: nc.vector.reduce_max(out=MXF.t[:], in_=NEC.t[:], axis=mybir.AxisListType.X),
                                 [NEC.buf], [MXF.buf])
                            k.cp(DVE, MXI.t[:], MXF.t[:], [MXF.buf], [MXI.buf])
                            k.dma(SP, CNT_d[0:1, 0:1], MXI.t[0:1, 0:1], [MXI.buf], [CNTb])

                    main_norm(l, 1, (SQ, RR(psum[6:8]), RT, RS), hf_cb)
                    pending.pop()()
                    k.barrier()
                    regs = nc.alloc_registers(f"cnt_{l}_{uid[0]}")
                    for reg in regs:
                        E = {mybir.EngineType.PE: PE, mybir.EngineType.DVE: DVE, mybir.EngineType.Activation: ACT,
                             mybir.EngineType.Pool: POOL, mybir.EngineType.SP: SP}[reg.engine]
                        k.wait(E, CNTb.w)
                        nc.reg_load(reg, CNT_d[0:1, 0:1])

                with contextlib.ExitStack() as st3:
                    HSE = Tile(sb("s_hse", [128, 5, D], BF16, st3))
                    HET = Tile(sb("s_het", [128, DC, CAPM], BF16, st3))
                    AT = tmp_pool(st3, "s_at", 2, BF16, (128, 4, CAPM))
                    SS = tmp_pool(st3, "s_ss", 2, F32, (128, CAPM))
                    YACC = Tile(sb("s_yacc", [128, 5, D], F32, st3))
                    PSG = RR(psum[0:6])
                    PSY = RR(psum[6:8])
                    all_hs = [HSb[tc][kk] for tc in range(TCH) for kk in range(2)]

                    def prep(e):
                        k.dma(SP, HSE.t[:], HS_d[e * CAPR:e * CAPR + CAPM, :].rearrange("(c p) f -> p c f", p=128),
                              all_hs, [HSE.buf])
                        for dc in range(DC):
                            ps = PSY.get()
                            psb = ps.t[:].bitcast(BF16)
                            for sc in range(5):
                                k.transpose(psb[:, sc * 128:(sc + 1) * 128], ps.buf, HSE.t[:, sc, dc * 128:(dc + 1) * 128],
                                            IDB.t[:], [HSE.buf, IDB.buf])
                            k.cp(ACT if dc % 2 == 0 else DVE, HET.t[:, dc, :], psb[:, 0:CAPM], [ps.buf], [HET.buf])

                    SA = 384
                    SB = CAPM - SA

                    def stage1(u):
                        slot, wg, wu = gu_l[u]
                        at = AT.get()
                        for fc in range(4):
                            pgA, puA, pB = PSG.get(), PSG.get(), PSG.get()
                            fs = slice(fc * 128, (fc + 1) * 128)
                            k.mm(pgA.t[:, 0:SA], pgA.buf, [(wg[:, kc, fs], HET.t[:, kc, 0:SA], [slot.buf, HET.buf])
                                                           for kc in range(DC)])
                            k.mm(puA.t[:, 0:SA], puA.buf, [(wu[:, kc, fs], HET.t[:, kc, 0:SA], [slot.buf, HET.buf])
                                                           for kc in range(DC)])
                            k.mm(pB.t[:, 0:SB], pB.buf, [(wg[:, kc, fs], HET.t[:, kc, SA:CAPM], [slot.buf, HET.buf])
                                                         for kc in range(DC)])
                            k.mm(pB.t[:, SB:2 * SB], pB.buf, [(wu[:, kc, fs], HET.t[:, kc, SA:CAPM], [slot.buf, HET.buf])
                                                              for kc in range(DC)])
                            s_ = SS.get()
                            k.act(s_.t[:, 0:SA], pgA.t[:, 0:SA], AF.Silu, [pgA.buf], [s_.buf])
                            k.act(s_.t[:, SA:CAPM], pB.t[:, 0:SB], AF.Silu, [pB.buf], [s_.buf])
                            k.tt(DVE, at.t[:, fc, 0:SA], s_.t[:, 0:SA], puA.t[:, 0:SA], ALU.mult, [s_.buf, puA.buf], [at.buf])
                            k.tt(DVE, at.t[:, fc, SA:CAPM], s_.t[:, SA:CAPM], pB.t[:, SB:2 * SB], ALU.mult,
                                 [s_.buf, pB.buf], [at.buf])
                        return at

                    def stage2(u, at):
                        slot, wd = wd_l[u]
                        blk = blocks[u]
                        for sc in range(5):
                            for hf_ in range(2):
                                py = PSY.get()
                                k.mm(py.t[:], py.buf, [(at.t[:, fc, sc * 128:(sc + 1) * 128],
                                                        wd[:, fc, hf_ * 512:(hf_ + 1) * 512], [at.buf, slot.buf])
                                                       for fc in range(4)])
                                ya = YACC.t[:, sc, hf_ * 512:(hf_ + 1) * 512]
                                if blk["bi"] == 0:
                                    k.cp(ACT, ya, py.t[:], [py.buf], [YACC.buf])
                                else:
                                    k.tt(DVE, ya, ya, py.t[:], ALU.add, [YACC.buf, py.buf], [YACC.buf])
                        if blk["bi"] == 3:
                            e = blk["e"]
                            k.dma(SP, YS_d[e * CAPM:(e + 1) * CAPM, :].rearrange("(c p) f -> p c f", p=128),
                                  YACC.t[:], [YACC.buf], [YSb[e]])

                    prep(0)
                    at_next = stage1(0)
                    load_gu(2)
                    for u in range(nb):
                        at = at_next
                        nxt_new_expert = (u + 1 < nb) and blocks[u + 1]["bi"] == 0
                        if nxt_new_expert:
                            prep(blocks[u + 1]["e"])
                            stage2(u, at)
                            load_wd(u + 2)
                            at_next = stage1(u + 1)
                            load_gu(u + 3)
                        else:
                            if u + 1 < nb:
                                at_next = stage1(u + 1)
                                load_gu(u + 3)
                            stage2(u, at)
                            load_wd(u + 2)
                    k.barrier()

                def fallback():
                    with contextlib.ExitStack() as sto:
                        SQ = tmp_pool(sto, "o_sq", 2, BF16)
                        RT = tmp_pool(sto, "o_rt", 1)
                        RS = tmp_pool(sto, "o_rs", 2)
                        GB = [Tile(sb(f"o_gb{i}", [128, S], F32, sto)) for i in range(2)]
                        GL = tmp_pool(sto, "o_gl", 2, F32, (128, 128))
                        main_norm(l, 1, (SQ, RR(psum[6:8]), RT, RS))
                        PSG2 = RR(psum[4:8])
                        gstate = {"i": 0}

                        def gate_mul(blk):
                            if blk["first"] and gstate.get("done") != blk["e"]:
                                gstate["done"] = blk["e"]
                                e = blk["e"]
                                G = GB[gstate["i"] % 2]
                                gstate["i"] += 1
                                for tt in range(NTT):
                                    ps = PSG2.get()
                                    for q in range(4):
                                        tc = tt * 4 + q
                                        gl = GL.get()
                                        k.ts(DVE, gl.t[:], ONES.t[:], GO.t[:, tc, e:e + 1], None, ALU.mult, None,
                                             [ONES.buf, GO.buf], [gl.buf])
                                        k.mm1(ps.t[:, q * 128:(q + 1) * 128], ps.buf, gl.t[:], ident, [gl.buf, CST.buf],
                                              True, True)
                                    k.cp(ACT, G.t[:, tt * 512:(tt + 1) * 512], ps.t[:], [ps.buf], [G.buf])
                                gstate["G"] = G
                            return gstate["G"]

                        blocks = []
                        for e in range(NE):
                            gv = w_eg[j, e].rearrange("(c p) f -> p c f", p=128)
                            uv = w_eu[j, e].rearrange("(c p) f -> p c f", p=128)
                            for f0 in range(0, DFE, 512):
                                blocks.append(dict(
                                    wg=gv[:, :, f0:f0 + 512], wu=uv[:, :, f0:f0 + 512],
                                    wd=w_ed[j, e, f0:f0 + 512, :].rearrange("(c p) d -> p c d", p=128), nfc=4,
                                    e=e, first=(f0 == 0)))
                        ffn_blocks(sto, blocks, gate_mul, slots=(GUS, WDS))
                        k.barrier()

                k.guarded(regs, OVT, fallback)

                with contextlib.ExitStack() as st4:
                    GA = tmp_pool(st4, "s_ga", 2, F32, (128, D))
                    GB_ = tmp_pool(st4, "s_gb", 2, F32, (128, D))
                    CB = tmp_pool(st4, "s_cb", 2, F32, (128, D))
                    PSC = RR(psum)
                    for tc in range(TCH):
                        tt = tc // 4
                        ga, gb = GA.get(), GB_.get()
                        k.idma(ga.t[:, :], None, YS_d[:, :], bass.IndirectOffsetOnAxis(ap=PJ1.t[:, tc:tc + 1], axis=0),
                               YSb + [PJ1.buf], [ga.buf])
                        k.idma(gb.t[:, :], None, YS_d[:, :], bass.IndirectOffsetOnAxis(ap=PJ2.t[:, tc:tc + 1], axis=0),
                               YSb + [PJ2.buf], [gb.buf])
                        cb = CB.get()
                        k.act(cb.t[:], ga.t[:], AF.Copy, [ga.buf, G1E.buf], [cb.buf], scale=G1E.t[:, tc:tc + 1])
                        k.stt(cb.t[:], gb.t[:], G2E.t[:, tc:tc + 1], cb.t[:], ALU.mult, ALU.add,
                              [gb.buf, G2E.buf, cb.buf], [cb.buf])
                        for h_ in range(2):
                            ps = PSC.get()
                            for q in range(4):
                                dc = h_ * 4 + q
                                k.transpose(ps.t[:, q * 128:(q + 1) * 128], ps.buf, cb.t[:, dc * 128:(dc + 1) * 128],
                                            ident, [cb.buf, CST.buf])
                            xv = XT[:, h_ * 4:(h_ + 1) * 4, tc * 128:(tc + 1) * 128]
                            k.tt(DVE, xv, xv, ps.t[:].rearrange("p (q d) -> p q d", q=4), ALU.add,
                                 [XTb[h_ * 4 + q][tt] for q in range(4)] + [ps.buf],
                                 [XTb[h_ * 4 + q][tt] for q in range(4)])
                k.barrier()

        def mla_layer(l, b):
            j = l // 2
            with contextlib.ExitStack() as st:
                W1 = Tile(sb("a_w1", [128, DC, D], BF16, st))
                HWP = tmp_pool(st, "a_hw", 2, BF16, (128, 1088))
                CQ = Tile(sb("a_cq", [128, 3, S], BF16, st))
                CKV = Tile(sb("a_ckv", [128, 2, S], BF16, st))
                T1 = Tile(sb("a_t1", [128, S], F32, st))
                T2 = Tile(sb("a_t2", [128, S], F32, st))
                QN = Tile(sb("a_qn", [128, S], BF16, st))
                QR = Tile(sb("a_qr", [128, S], BF16, st))
                KN = Tile(sb("a_kn", [128, S], BF16, st))
                KR = Tile(sb("a_kr", [128, S], BF16, st))
                VV = Tile(sb("a_v", [128, TCH, 128], BF16, st))
                SQ = tmp_pool(st, "a_sq", 2, BF16)
                RT = tmp_pool(st, "a_rt", 1)
                RS = tmp_pool(st, "a_rs", 2)
                TA = tmp_pool(st, "a_ta", 6)
                PT = tmp_pool(st, "a_pt", 5, BF16, (128, 512))
                PI = Tile(sb("a_pi", [64, 512], I32, st))
                KI = Tile(sb("a_ki", [64, 512], I32, st))

                wd_v = w_mdown[j].rearrange("(c p) f -> p c f", p=128)
                for c in range(DC):
                    k.dma(POOL, W1.t[:, c, 0:704], wd_v[:, c, :], [], [W1.buf])
                wuq_v = w_muq[j].rearrange("(c p) f -> p c f", p=128)
                wukv_v = w_mukv[j].rearrange("(c p) f -> p c f", p=128)

                def load_head(hd):
                    hw = HWP.get()
                    k.dma(POOL, hw.t[:, 0:576].rearrange("p (c f) -> p c f", c=3),
                          wuq_v[:, :, hd * 192:(hd + 1) * 192], [], [hw.buf])
                    k.dma(POOL, hw.t[:, 576:1088].rearrange("p (c f) -> p c f", c=2),
                          wukv_v[:, :, hd * 256:(hd + 1) * 256], [], [hw.buf])
                    return hw

                hws = {0: load_head(0), 1: load_head(1)}
                ZPAD = Buf()
                k.op(POOL, lambda: nc.gpsimd.memset(QR.t[64:128, :], 0.0), [], [ZPAD])
                k.op(POOL, lambda: nc.gpsimd.memset(KR.t[64:128, :], 0.0), [], [ZPAD])

                PIS = [PI, KI]
                T2b, T1cb = Buf(), Buf()

                PTMP = RR([Tile.__new__(Tile) for _ in range(4)])
                for i_, tl in enumerate(PTMP.items):
                    src_t = QN if i_ < 2 else KN
                    tl.t = src_t.t[:, (i_ % 2) * 1024:(i_ % 2 + 1) * 1024].bitcast(F32)
                    tl.buf = Buf()

                def reduce_angle(E, a, abuf):
                    TP = TA if E is DVE else PTMP

                    def fma(x, c):
                        if E is DVE:
                            k.stt(a, x, c, a, ALU.mult, ALU.add, [kf.buf, abuf], [abuf])
                        else:
                            tm = TP.get()
                            k.ts(E, tm.t[0:64, :], x, c, None, ALU.mult, None, [kf.buf], [tm.buf])
                            k.tt(E, a, tm.t[0:64, :], a, ALU.add, [tm.buf, abuf], [abuf])

                    ki = TP.get()
                    ki_i = ki.t[0:64, :].bitcast(I32)
                    k.ts(E, ki_i, a, float(1.0 / (2 * np.pi)), None, ALU.mult, None, [abuf], [ki.buf])
                    kf = TP.get()
                    k.cp(E, kf.t[0:64, :], ki_i, [ki.buf], [kf.buf])
                    fma(kf.t[0:64, :], -6.28125)
                    fma(kf.t[0:64, :], -0.0019353071795864769)
                    k.ts(E, kf.t[0:64, :], a, float(np.pi), None, ALU.is_gt, None, [abuf], [kf.buf])
                    fma(kf.t[0:64, :], float(-2 * np.pi))
                    k.ts(E, kf.t[0:64, :], a, float(-np.pi), None, ALU.is_lt, None, [abuf], [kf.buf])
                    fma(kf.t[0:64, :], float(2 * np.pi))
                    k.ts(E, a, a, -3.1415925, 3.1415925, ALU.max, ALU.min, [abuf], [abuf])

                def tables(tt):
                    cols = slice(tt * 512, (tt + 1) * 512)
                    pi_t = PIS[tt % 2]
                    k.dma(SP, pi_t.t[:], pos_d[b:b + 1, cols].partition_broadcast(64), [], [pi_t.buf])
                    pf = TA.get()
                    k.cp(DVE, pf.t[0:64, :], pi_t.t[:], [pi_t.buf], [pf.buf])
                    k.ts(DVE, T2.t[0:64, cols], pf.t[0:64, :], gcol(GC_FREQ, 0, 64), None, ALU.mult, None,
                         [pf.buf, GC.buf], [T2b])
                    k.ts(DVE, T1.t[0:64, cols], pf.t[0:64, :], gcol(GC_FREQ, 0, 64), float(np.pi / 2),
                         ALU.mult, ALU.add, [pf.buf, GC.buf], [T1cb])
                    reduce_angle(DVE, T1.t[0:64, cols], T1cb)
                    reduce_angle(DVE, T2.t[0:64, cols], T2b)

                main_norm(l, 0, (SQ, RR(psum[6:8]), RT, RS))

                PSA = RR(psum[0:6])
                PSS = RR(psum[6:8])
                for tt in range(NTT):
                    cols = slice(tt * 512, (tt + 1) * 512)

                    def down(m0, m1):
                        ps = PSA.get()
                        k.mm(ps.t[0:m1 - m0, :], ps.buf, [(W1.t[:, kc, m0:m1], HT[:, kc, cols], [W1.buf, HTb[kc][tt]])
                                                          for kc in range(DC)])
                        return ps
                    dq = [down(c * 128, (c + 1) * 128) for c in range(3)]
                    rs = rms_stats((SQ, PSS, RT, RS), [(p.t[:], p.buf, 0, 128) for p in dq], QL)
                    for c in range(3):
                        k.stt(CQ.t[:, c, cols], dq[c].t[:], gcol(GC_QA + j * 3 + c), rs.t[:], ALU.mult, ALU.mult,
                              [dq[c].buf, GC.buf, rs.buf], [CQ.buf])
                    dkv = [down(QL + c * 128, QL + (c + 1) * 128) for c in range(2)]
                    pkr = down(QL + KVL, QL + KVL + ROPE)
                    rs = rms_stats((SQ, PSS, RT, RS), [(p.t[:], p.buf, 0, 128) for p in dkv], KVL)
                    for c in range(2):
                        k.stt(CKV.t[:, c, cols], dkv[c].t[:], gcol(GC_KVA + j * 2 + c), rs.t[:], ALU.mult, ALU.mult,
                              [dkv[c].buf, GC.buf, rs.buf], [CKV.buf])
                    k.cp(DVE, T1.t[64:128, cols], pkr.t[0:64, :], [pkr.buf], [T1.buf])
                    tables(tt)
                for tt in range(NTT):
                    cols = slice(tt * 512, (tt + 1) * 512)
                    k.act(T1.t[0:64, cols], T1.t[0:64, cols], AF.Sin, [T1cb], [T1cb])
                    k.act(T2.t[0:64, cols], T2.t[0:64, cols], AF.Sin, [T2b], [T2b])
                    k.ts(DVE, T2.t[0:64, cols], T2.t[0:64, cols], gcol(GC_SIGN, 0, 64), None, ALU.mult, None,
                         [T2b, GC.buf], [T2b])

                wo_v = w_mo[j].rearrange("(c p) f -> p c f", p=128)
                for c in range(DC):
                    k.dma(POOL, W1.t[:, c, :], wo_v[:, c, :], [], [W1.buf])

                QNb = [Buf() for _ in range(NTT)]
                QRb = [Buf() for _ in range(NTT)]
                KNb = [Buf() for _ in range(NTT)]
                KRb = [Buf() for _ in range(NTT)]
                VVb = [Buf() for _ in range(NTT)]

                def rope(E, src, dst, dbuf, cols):
                    ra = TA.get()
                    k.tt(E, ra.t[0:64, :], src.t[0:64, :], T1.t[0:64, cols], ALU.mult, [src.buf, T1cb], [ra.buf])
                    rb = TA.get()
                    k.tt(E, rb.t[0:32, :], src.t[32:64, :], T2.t[32:64, cols], ALU.mult, [src.buf, T2b], [rb.buf])
                    k.tt(E, rb.t[32:64, :], src.t[0:32, :], T2.t[0:32, cols], ALU.mult, [src.buf, T2b], [rb.buf])
                    k.tt(E, dst.t[0:64, cols], ra.t[0:64, :], rb.t[0:64, :], ALU.add, [ra.buf, rb.buf], [dbuf])

                PSP = RR(psum[0:4])
                PSS2 = RR(psum[4:6])
                gqn, gqr = GC_QN + j * 2, GC_QN + j * 2 + 1
                gkn, gkr = GC_KN + j * 2, GC_KN + j * 2 + 1

                def proj(hw, tt):
                    WUQ = hw.t[:, 0:576].rearrange("p (c f) -> p c f", c=3)
                    WUKV = hw.t[:, 576:1088].rearrange("p (c f) -> p c f", c=2)
                    cols = slice(tt * 512, (tt + 1) * 512)
                    pa, pb_, pc = PSP.get(), PSP.get(), PSP.get()
                    k.mm(pa.t[:], pa.buf, [(WUQ[:, kc, 0:128], CQ.t[:, kc, cols], [hw.buf, CQ.buf])
                                           for kc in range(3)])
                    k.mm(pb_.t[0:64, :], pb_.buf, [(WUQ[:, kc, 128:192], CQ.t[:, kc, cols], [hw.buf, CQ.buf])
                                                    for kc in range(3)])
                    k.mm(pc.t[:], pc.buf, [(WUKV[:, kc, 0:128], CKV.t[:, kc, cols], [hw.buf, CKV.buf])
                                           for kc in range(2)])
                    pv = PSS2.get()
                    for q in range(4):
                        tc = tt * 4 + q
                        k.mm(pv.t[:, q * 128:(q + 1) * 128], pv.buf,
                             [(CKV.t[:, kc, tc * 128:(tc + 1) * 128], WUKV[:, kc, 128:256], [CKV.buf, hw.buf])
                              for kc in range(2)])
                    k.cp(ACT, VV.t[:, tt * 4:(tt + 1) * 4, :], pv.t[:].rearrange("p (q d) -> p q d", q=4),
                         [pv.buf], [VVb[tt]])
                    rs = rms_stats((SQ, PSS2, RT, RS), [(pa.t[:], pa.buf, 0, 128), (pb_.t[0:64, :], pb_.buf, 0, 64)],
                                   192)
                    k.stt(QN.t[:, cols], pa.t[:], gcol(gqn), rs.t[:], ALU.mult, ALU.mult,
                          [pa.buf, GC.buf, rs.buf, T1cb, T2b], [QNb[tt]])
                    tq = TA.get()
                    k.stt(tq.t[0:64, :], pb_.t[0:64, :], gcol(gqr, 0, 64), rs.t[0:64, :], ALU.mult, ALU.mult,
                          [pb_.buf, GC.buf, rs.buf], [tq.buf])
                    rope(DVE, tq, QR, QRb[tt], cols)
                    rs = rms_stats((SQ, PSS2, RT, RS), [(pc.t[:], pc.buf, 0, 128),
                                                        (T1.t[64:128, cols], T1.buf, 64, 128)], 192)
                    k.stt(KN.t[:, cols], pc.t[:], gcol(gkn), rs.t[:], ALU.mult, ALU.mult,
                          [pc.buf, GC.buf, rs.buf, T1cb, T2b], [KNb[tt]])
                    tk = TA.get()
                    k.stt(tk.t[0:64, :], T1.t[64:128, cols], gcol(gkr, 64, 128), rs.t[64:128, :],
                          ALU.mult, ALU.mult, [T1.buf, GC.buf, rs.buf], [tk.buf])
                    rope(POOL, tk, KR, KRb[tt], cols)

                def attn(hd, jt):
                    qcols0 = jt * 512
                    po, pl = (psum[4], psum[5]) if jt % 2 == 0 else (psum[6], psum[7])
                    nck = 4 * (jt + 1)

                    def score(c):
                        i = c - 4 * jt
                        q_lo = 128 * i if i > 0 else 0
                        n = 512 - q_lo
                        qs = slice(qcols0 + q_lo, qcols0 + 512)
                        ks = slice(c * 128, (c + 1) * 128)
                        kt = c // 4
                        pS = PSP.get()
                        k.mm(pS.t[:, 0:n], pS.buf, [(KN.t[:, ks], QN.t[:, qs], [KNb[kt], QNb[jt]]),
                                                    (KR.t[:, ks], QR.t[:, qs], [KRb[kt], QRb[jt], ZPAD])])
                        pt = PT.get()
                        k.act(pt.t[:, 0:n], pS.t[:, 0:n], AF.Exp, [pS.buf], [pt.buf])
                        if i >= 0:
                            k.tt(DVE, pt.t[:, 0:128], pt.t[:, 0:128], TRI.t[:], ALU.mult, [pt.buf, TRI.buf], [pt.buf])
                        return pt, q_lo, n

                    LOOK = 3
                    pend = [score(c) for c in range(min(LOOK, nck))]
                    for c in range(nck):
                        pt, q_lo, n = pend.pop(0)
                        if c + LOOK < nck:
                            pend.append(score(c + LOOK))
                        k.mm1(po.t[:, q_lo:512], po.buf, VV.t[:, c, :], pt.t[:, 0:n], [VVb[c // 4], pt.buf],
                              c == 0, c == nck - 1)
                        k.mm1(pl.t[:, q_lo:512], pl.buf, ONES_B.t[:], pt.t[:, 0:n], [ONES_B.buf, pt.buf],
                              c == 0, c == nck - 1)
                    rl = TA.get()
                    k.act(rl.t[:], pl.t[:], AF.Ln, [pl.buf], [rl.buf])
                    k.act(rl.t[:], rl.t[:], AF.Exp, [rl.buf], [rl.buf], scale=-1.0)
                    k.tt(DVE, HT[:, hd, qcols0:qcols0 + 512], po.t[:], rl.t[:], ALU.mult, [po.buf, rl.buf],
                         [HTb[hd][jt]])

                for hd in range(NH):
                    if hd + 1 < NH and (hd + 1) not in hws:
                        hws[hd + 1] = load_head(hd + 1)
                    hw = hws.pop(hd)
                    proj(hw, 0)
                    for tt in range(1, NTT):
                        proj(hw, tt)
                        attn(hd, tt - 1)
                    attn(hd, NTT - 1)
                PSY = RR(psum[0:4])
                for tt in range(NTT):
                    cols = slice(tt * 512, (tt + 1) * 512)
                    for dm in range(DC):
                        py = PSY.get()
                        k.mm(py.t[:], py.buf, [(W1.t[:, hc, dm * 128:(dm + 1) * 128], HT[:, hc, cols],
                                                [W1.buf, HTb[hc][tt]]) for hc in range(DC)])
                        add_to_x(dm, tt, py)
                k.barrier()

        ONES_B = Tile(sb("ONESB", [128, 128], BF16))
        k.op(DVE, lambda: nc.vector.memset(ONES_B.t[:], 1.0), [], [ONES_B.buf])
        IDB = Tile(sb("IDB", [128, 128], BF16))
        k.cp(DVE, IDB.t[:], CST.t[:, 0:128], [CST.buf], [IDB.buf])
        k.barrier()

        for b in range(n_seq):
            load_x(b)
            for l in layer_ids:
                if l % 2 == 0:
                    if not SKIP_MIX:
                        conv_layer(l)
                    if not SKIP_FFN:
                        dense_ffn_layer(l)
                else:
                    if not SKIP_MIX:
                        mla_layer(l, b)
                    if not SKIP_FFN:
                        if SPARSE:
                            moe_layer_sparse(l)
                        else:
                            moe_layer(l)
            store_x(b)
        k.barrier()
    return nc


def prep_consts(inputs):
    g = np.zeros((128, NG), np.float32)
    nm = np.asarray(inputs["norm_mix"], np.float32)
    nf = np.asarray(inputs["norm_ffn"], np.float32)
    for l in range(DEPTH):
        g[:, GC_NMIX + l * DC: GC_NMIX + (l + 1) * DC] = nm[l].reshape(DC, 128).T
        g[:, GC_NFFN + l * DC: GC_NFFN + (l + 1) * DC] = nf[l].reshape(DC, 128).T
    cw = np.asarray(inputs["conv_w"], np.float32)
    for j in range(2):
        for kk in range(3):
            base = GC_CONV + (j * 3 + kk) * DC
            g[:, base:base + DC] = cw[j, kk].reshape(DC, 128).T
    qa = np.asarray(inputs["mla_q_a_norm"], np.float32)
    kva = np.asarray(inputs["mla_kv_a_norm"], np.float32)
    qn = np.asarray(inputs["mla_q_norm"], np.float32)
    kn = np.asarray(inputs["mla_k_norm"], np.float32)
    for j in range(2):
        g[:, GC_QA + j * 3: GC_QA + j * 3 + 3] = qa[j].reshape(3, 128).T
        g[:, GC_KVA + j * 2: GC_KVA + j * 2 + 2] = kva[j].reshape(2, 128).T
        g[:, GC_QN + j * 2] = qn[j, 0:128]
        g[0:64, GC_QN + j * 2 + 1] = qn[j, 128:192]
        g[:, GC_KN + j * 2] = kn[j, 0:128]
        g[64:128, GC_KN + j * 2 + 1] = kn[j, 128:192]
    inv_freq = (np.float32(10000.0) ** (-np.arange(0, ROPE, 2, dtype=np.float32) / np.float32(ROPE))).astype(np.float32)
    g[0:32, GC_FREQ] = inv_freq
    g[32:64, GC_FREQ] = inv_freq
    g[0:32, GC_SIGN] = 1.0
    g[32:64, GC_SIGN] = -1.0
    cst = np.zeros((128, 640), np.float32)
    cst[:, 512:640] = np.tile(np.arange(NE, dtype=np.float32) * CAPM, 16)[None, :]
    cst[:, 256:384] = (kk_ := np.arange(128)[:, None] < np.arange(128)[None, :]).astype(np.float32)
    cst[:, 384:512] = np.tile(np.arange(NE, dtype=np.float32) * CAPR, 16)[None, :]
    cst[:, 0:128] = np.eye(128, dtype=np.float32)
    kk_, qq_ = np.meshgrid(np.arange(128), np.arange(128), indexing="ij")
    cst[:, 128:256] = (qq_ >= kk_).astype(np.float32)
    return g, cst


W_NAMES = ["conv_w_in", "conv_w_out", "mla_w_down", "mla_w_uq", "mla_w_ukv", "mla_w_o",
           "ffn_w_gate", "ffn_w_up", "ffn_w_down", "moe_router", "moe_w_gate", "moe_w_up", "moe_w_down"]


def kernel(**inputs):
    x = np.ascontiguousarray(np.asarray(inputs["x"], np.float32))
    pos = np.ascontiguousarray(np.asarray(inputs["positions"], np.int32))
    g, cst = prep_consts(inputs)
    shared = {n: np.ascontiguousarray(np.asarray(inputs[n], np.float32)) for n in W_NAMES}
    shared["gcols"] = g
    shared["cst"] = cst
    nc = build_program(list(range(DEPTH)), SEQ_PER_CORE)
    in_maps = []
    for c in range(N_CORES):
        m = dict(shared)
        m["x"] = x[c * SEQ_PER_CORE:(c + 1) * SEQ_PER_CORE]
        m["positions"] = pos[c * SEQ_PER_CORE:(c + 1) * SEQ_PER_CORE]
        in_maps.append(m)
    res = run_bass_kernel_spmd(nc, in_maps, core_ids=list(range(N_CORES)))
    return np.concatenate([np.asarray(r["out"], np.float32) for r in res.results], axis=0)
```
